# Optimizing a Trainium2 kernel written in Bass

```python
import math
import jax
import jax.numpy as jnp
from jax import lax
import numpy as np

D_MODEL = 1024
BATCH = 4
SEQ = 4096
DEPTH = 1

CHUNK = 64
Q_BLOCK = 128
ROPE_THETA = 500000.0
EPS = 1e-6

MLA_HEADS = 8
MLA_NOPE = 64
MLA_ROPE = 32
MLA_V = 64
MLA_QK = MLA_NOPE + MLA_ROPE
MLA_Q_RANK = 256
MLA_KV_RANK = 128
MLA_V_WIDTH = MLA_HEADS * MLA_V

DIFF_HEADS = 4
DIFF_HEAD_DIM = 64
DIFF_V_DIM = 2 * DIFF_HEAD_DIM
DIFF_ROPE = DIFF_HEAD_DIM // 4
DIFF_QK_WIDTH = DIFF_HEADS * 2 * DIFF_HEAD_DIM
DIFF_V_WIDTH = DIFF_HEADS * DIFF_V_DIM

N_GROUPS = 4
EXPERTS_PER_GROUP = 8
N_EXPERTS = N_GROUPS * EXPERTS_PER_GROUP
TOP_K = 2
EXPERT_FF = 256

IN_SIZES = (MLA_Q_RANK, MLA_KV_RANK, MLA_ROPE, DIFF_QK_WIDTH, DIFF_QK_WIDTH, DIFF_V_WIDTH, D_MODEL, D_MODEL)
IN_WIDTH = sum(IN_SIZES)
IN_SPLITS = tuple(int(s) for s in np.cumsum(IN_SIZES)[:-1])

kernel_name = 'hybrid_mla_diffattn_hiermoe_block'


def rms_norm(x, gain):
    x32 = x.astype(jnp.float32)
    y = x32 * lax.rsqrt(jnp.mean(x32 * x32, axis=-1, keepdims=True) + EPS)
    return (y * gain.astype(jnp.float32)).astype(x.dtype)


def rope_tables(seq_len, rot_dim):
    pos = jnp.arange(seq_len, dtype=jnp.float32)
    inv = 1.0 / (ROPE_THETA ** (jnp.arange(0, rot_dim, 2, dtype=jnp.float32) / rot_dim))
    ang = pos[:, None] * inv[None, :]
    return jnp.cos(ang), jnp.sin(ang)


def apply_rope(x, cos, sin):
    shape = (1, cos.shape[0]) + (1,) * (x.ndim - 3) + (cos.shape[1],)
    c = cos.reshape(shape)
    s = sin.reshape(shape)
    x32 = x.astype(jnp.float32)
    x1, x2 = jnp.split(x32, 2, axis=-1)
    return jnp.concatenate([x1 * c - x2 * s, x2 * c + x1 * s], axis=-1).astype(x.dtype)


def chunk_causal_probs(q_blk, k_ctx, q_start, scale):
    n_q = q_blk.shape[1]
    n_k = k_ctx.shape[1]
    s = jnp.einsum('bqhd,bkhd->bhqk', q_blk.astype(jnp.float32), k_ctx.astype(jnp.float32)) * scale
    q_chunk = (q_start + jnp.arange(n_q)) // CHUNK
    k_chunk = jnp.arange(n_k) // CHUNK
    s = jnp.where(k_chunk[None, :] <= q_chunk[:, None], s, -jnp.inf)
    return jax.nn.softmax(s, axis=-1)


def mla_branch(q_lat, kv_lat, k_rope_in, q_lat_norm, w_uq, kv_lat_norm, w_ukv, q_gain, k_gain, cos, sin):
    b, s, _ = q_lat.shape
    q = (rms_norm(q_lat, q_lat_norm) @ w_uq).reshape(b, s, MLA_HEADS, MLA_QK)
    kv = (rms_norm(kv_lat, kv_lat_norm) @ w_ukv).reshape(b, s, MLA_HEADS, MLA_NOPE + MLA_V)
    k_nope, v = kv[..., :MLA_NOPE], kv[..., MLA_NOPE:]
    k_rope = jnp.broadcast_to(k_rope_in[:, :, None, :], (b, s, MLA_HEADS, MLA_ROPE))
    k = jnp.concatenate([k_nope, k_rope], axis=-1)
    q = rms_norm(q, q_gain)
    k = rms_norm(k, k_gain)
    q = jnp.concatenate([q[..., :MLA_NOPE], apply_rope(q[..., MLA_NOPE:], cos, sin)], axis=-1)
    k = jnp.concatenate([k[..., :MLA_NOPE], apply_rope(k[..., MLA_NOPE:], cos, sin)], axis=-1)
    scale = MLA_QK ** -0.5
    outs = []
    for start in range(0, s, Q_BLOCK):
        end = start + Q_BLOCK
        p = chunk_causal_probs(q[:, start:end], k[:, :end], start, scale)
        outs.append(jnp.einsum('bhqk,bkhd->bqhd', p, v[:, :end].astype(jnp.float32)))
    o = jnp.concatenate(outs, axis=1).astype(q_lat.dtype)
    return o.reshape(b, s, MLA_V_WIDTH)


def diff_branch(q_in, k_in, v_in, q_gain, k_gain, lq1, lk1, lq2, lk2, subln, lambda_init, cos, sin):
    b, s, _ = q_in.shape
    q = rms_norm(q_in.reshape(b, s, DIFF_HEADS, 2, DIFF_HEAD_DIM), q_gain)
    k = rms_norm(k_in.reshape(b, s, DIFF_HEADS, 2, DIFF_HEAD_DIM), k_gain)
    v = v_in.reshape(b, s, DIFF_HEADS, DIFF_V_DIM)
    q = jnp.concatenate([apply_rope(q[..., :DIFF_ROPE], cos, sin), q[..., DIFF_ROPE:]], axis=-1)
    k = jnp.concatenate([apply_rope(k[..., :DIFF_ROPE], cos, sin), k[..., DIFF_ROPE:]], axis=-1)
    q1, q2 = q[:, :, :, 0], q[:, :, :, 1]
    k1, k2 = k[:, :, :, 0], k[:, :, :, 1]
    lam = (jnp.exp(jnp.sum(lq1.astype(jnp.float32) * lk1.astype(jnp.float32)))
           - jnp.exp(jnp.sum(lq2.astype(jnp.float32) * lk2.astype(jnp.float32))) + lambda_init)
    scale = DIFF_HEAD_DIM ** -0.5
    outs = []
    for start in range(0, s, Q_BLOCK):
        end = start + Q_BLOCK
        p1 = chunk_causal_probs(q1[:, start:end], k1[:, :end], start, scale)
        p2 = chunk_causal_probs(q2[:, start:end], k2[:, :end], start, scale)
        a = p1 - lam * p2
        outs.append(jnp.einsum('bhqk,bkhd->bqhd', a, v[:, :end].astype(jnp.float32)))
    o = jnp.concatenate(outs, axis=1)
    o = rms_norm(o, subln) * (1.0 - lambda_init)
    return o.astype(q_in.dtype).reshape(b, s, DIFF_V_WIDTH)


def hier_moe(h, w_rg, b_rg, w_re, b_re, w_gate, w_up, w_down):
    b, s, d = h.shape
    t = h.reshape(b * s, d)
    n = t.shape[0]
    group_logits = (t @ w_rg).astype(jnp.float32) + b_rg.astype(jnp.float32)
    p_group = jax.nn.softmax(group_logits, axis=-1)
    pg_sel, g_idx = lax.top_k(p_group, 1)
    exp_logits = ((t @ w_re).astype(jnp.float32) + b_re.astype(jnp.float32)).reshape(n, N_GROUPS, EXPERTS_PER_GROUP)
    in_group = jnp.take_along_axis(exp_logits, g_idx[:, :, None], axis=1)[:, 0]
    p_exp = jax.nn.softmax(in_group, axis=-1)
    pe_top, e_idx = lax.top_k(p_exp, TOP_K)
    weights = pg_sel * pe_top / jnp.sum(pe_top, axis=-1, keepdims=True)
    global_idx = g_idx * EXPERTS_PER_GROUP + e_idx
    combine = jnp.sum(jax.nn.one_hot(global_idx, N_EXPERTS, dtype=jnp.float32) * weights[..., None], axis=1)
    out = jnp.zeros((n, d), jnp.float32)
    for e in range(N_EXPERTS):
        hidden = jax.nn.silu(t @ w_gate[e]) * (t @ w_up[e])
        out = out + combine[:, e:e + 1] * (hidden @ w_down[e]).astype(jnp.float32)
    return out.astype(h.dtype).reshape(b, s, d)


def setup_inputs(seed: int = 0) -> dict:
    key = jax.random.key(seed)
    ks = jax.random.split(key, 32)
    f32 = jnp.float32
    L = DEPTH

    def w(k, shape, fan_in):
        return jax.random.normal(k, shape, f32) * (fan_in ** -0.5)

    def gain(k, shape):
        return 1.0 + 0.02 * jax.random.normal(k, shape, f32)

    def small(k, shape, sc):
        return sc * jax.random.normal(k, shape, f32)

    return {
        'x': jax.random.normal(ks[0], (BATCH, SEQ, D_MODEL), f32),
        'norm_mix': gain(ks[1], (L, D_MODEL)),
        'w_in': w(ks[2], (L, D_MODEL, IN_WIDTH), D_MODEL),
        'mla_q_latent_norm': gain(ks[3], (L, MLA_Q_RANK)),
        'w_mla_uq': w(ks[4], (L, MLA_Q_RANK, MLA_HEADS * MLA_QK), MLA_Q_RANK),
        'mla_kv_latent_norm': gain(ks[5], (L, MLA_KV_RANK)),
        'w_mla_ukv': w(ks[6], (L, MLA_KV_RANK, MLA_HEADS * (MLA_NOPE + MLA_V)), MLA_KV_RANK),
        'mla_q_gain': gain(ks[7], (L, MLA_QK)),
        'mla_k_gain': gain(ks[8], (L, MLA_QK)),
        'diff_q_gain': gain(ks[9], (L, DIFF_HEAD_DIM)),
        'diff_k_gain': gain(ks[10], (L, DIFF_HEAD_DIM)),
        'lambda_q1': small(ks[11], (L, DIFF_HEAD_DIM), 0.1),
        'lambda_k1': small(ks[12], (L, DIFF_HEAD_DIM), 0.1),
        'lambda_q2': small(ks[13], (L, DIFF_HEAD_DIM), 0.1),
        'lambda_k2': small(ks[14], (L, DIFF_HEAD_DIM), 0.1),
        'diff_subln': gain(ks[15], (L, DIFF_V_DIM)),
        'w_mla_up': w(ks[16], (L, MLA_V_WIDTH, D_MODEL), MLA_V_WIDTH),
        'w_diff_up': w(ks[17], (L, DIFF_V_WIDTH, D_MODEL), DIFF_V_WIDTH),
        'w_out': w(ks[18], (L, D_MODEL, D_MODEL), D_MODEL),
        'norm_ffn': gain(ks[19], (L, D_MODEL)),
        'w_router_group': w(ks[20], (L, D_MODEL, N_GROUPS), D_MODEL),
        'b_router_group': small(ks[21], (L, N_GROUPS), 0.01),
        'w_router_expert': w(ks[22], (L, D_MODEL, N_EXPERTS), D_MODEL),
        'b_router_expert': small(ks[23], (L, N_EXPERTS), 0.01),
        'w_expert_gate': w(ks[24], (L, N_EXPERTS, D_MODEL, EXPERT_FF), D_MODEL),
        'w_expert_up': w(ks[25], (L, N_EXPERTS, D_MODEL, EXPERT_FF), D_MODEL),
        'w_expert_down': w(ks[26], (L, N_EXPERTS, EXPERT_FF, D_MODEL), EXPERT_FF),
    }


def reference(x, norm_mix, w_in, mla_q_latent_norm, w_mla_uq, mla_kv_latent_norm, w_mla_ukv,
              mla_q_gain, mla_k_gain, diff_q_gain, diff_k_gain, lambda_q1, lambda_k1, lambda_q2, lambda_k2,
              diff_subln, w_mla_up, w_diff_up, w_out, norm_ffn, w_router_group, b_router_group,
              w_router_expert, b_router_expert, w_expert_gate, w_expert_up, w_expert_down):
    seq_len = x.shape[1]
    cos_mla, sin_mla = rope_tables(seq_len, MLA_ROPE)
    cos_diff, sin_diff = rope_tables(seq_len, DIFF_ROPE)
    for l in range(DEPTH):
        lambda_init = 0.8 - 0.6 * math.exp(-0.3 * l)
        h = rms_norm(x, norm_mix[l])
        proj = h @ w_in[l]
        q_lat, kv_lat, k_rope_in, dq, dk, dv, g_mla, g_diff = jnp.split(proj, IN_SPLITS, axis=-1)
        o_mla = mla_branch(q_lat, kv_lat, k_rope_in, mla_q_latent_norm[l], w_mla_uq[l],
                           mla_kv_latent_norm[l], w_mla_ukv[l], mla_q_gain[l], mla_k_gain[l],
                           cos_mla, sin_mla)
        o_diff = diff_branch(dq, dk, dv, diff_q_gain[l], diff_k_gain[l], lambda_q1[l], lambda_k1[l],
                             lambda_q2[l], lambda_k2[l], diff_subln[l], lambda_init, cos_diff, sin_diff)
        merged = (jax.nn.sigmoid(g_mla) * (o_mla @ w_mla_up[l])
                  + jax.nn.sigmoid(g_diff) * (o_diff @ w_diff_up[l]))
        x = x + merged @ w_out[l]
        h2 = rms_norm(x, norm_ffn[l])
        x = x + hier_moe(h2, w_router_group[l], b_router_group[l], w_router_expert[l], b_router_expert[l],
                         w_expert_gate[l], w_expert_up[l], w_expert_down[l])
    return x
```

```python
import contextlib
import numpy as np
import concourse.bass as bass
import concourse.mybir as mybir
from concourse.bass_utils import run_bass_kernel_spmd

F32 = mybir.dt.float32
BF = mybir.dt.bfloat16
I32 = mybir.dt.int32
AF = mybir.ActivationFunctionType
ALU = mybir.AluOpType
AX = mybir.AxisListType

ENGS = ("pe", "act", "dve", "pool", "sp")
NSLOT = 8
NSLOTS = {"sp": 16, "pool": 32}
EPOCH = 12000
EPS = 1e-6
LAMBDA_INIT = 0.2
DEBUG = False
USE_POW = False
VERBOSE = False
NT = 48
BIG = 1.0e6
WSPLIT = 1


class GTag:
    __slots__ = ("key", "gen")

    def __init__(self, key, gen):
        self.key, self.gen = key, gen


def norm_tag(t):
    if isinstance(t, GTag):
        return t.key, t.gen
    if isinstance(t, tuple):
        ks, g = [], 0
        for x in t:
            k, gg = norm_tag(x)
            ks.append(k)
            g = max(g, gg)
        return tuple(ks), g
    return t, 0


class Op:
    __slots__ = ("eng", "idx", "fn", "deps", "dma", "sig", "needs", "reads", "writes", "lvl")

    def __init__(self, eng, idx, fn, deps, dma):
        self.eng, self.idx, self.fn, self.deps, self.dma = eng, idx, fn, deps, dma
        self.sig = None
        self.needs = False
        self.lvl = 0


class Sched:
    def __init__(self, nc):
        self.nc = nc
        self.ops = {e: [] for e in ENGS}
        self.last_w = {}
        self.readers = {}
        self.recent_dma = {e: [] for e in ENGS}
        self.rec = None
        self.gen = {}

    def op(self, eng, fn, reads=(), writes=(), dma=False):
        o = Op(eng, -1, fn, set(), dma)
        o.reads = [norm_tag(t) for t in reads]
        o.writes = [norm_tag(t) for t in writes]
        if self.rec is not None:
            self.rec.append(o)
            return o
        return self.commit(o)

    def commit(self, o):
        eng = o.eng
        deps = o.deps
        for t, g in o.reads:
            cg = self.gen.get(t)
            assert cg is None or cg == g, f"ring too shallow: read of {t} gen {g} but current gen {cg}"
            w = self.last_w.get(t)
            if w is not None:
                deps.add(w)
        for t, g in o.writes:
            cg = self.gen.get(t)
            assert cg is None or cg <= g, f"ring too shallow: write of {t} gen {g} but current gen {cg}"
            self.gen[t] = g
            w = self.last_w.get(t)
            if w is not None:
                deps.add(w)
            for r in self.readers.get(t, ()):
                deps.add(r)
        deps.discard(o)
        o.idx = len(self.ops[eng])
        for t, g in o.reads:
            self.readers.setdefault(t, []).append(o)
        for t, g in o.writes:
            self.last_w[t] = o
            self.readers[t] = []
        self.ops[eng].append(o)
        if o.dma:
            rd = self.recent_dma[eng]
            rd.append(o)
            if len(rd) > NSLOTS.get(eng, NSLOT):
                rd.pop(0)
        return o

    def auto_pipeline(self, n, item_fn, extra=None):
        recs = []
        maxl = 0
        for i in range(n):
            self.rec = []
            item_fn(i)
            ops, self.rec = self.rec, None
            lw, rd, le = {}, {}, {}
            for o in ops:
                l = le.get(o.eng, 0)
                for t, g in o.reads:
                    w = lw.get(t)
                    if w is not None:
                        l = max(l, w.lvl + (w.eng != o.eng))
                for t, g in o.writes:
                    w = lw.get(t)
                    if w is not None:
                        l = max(l, w.lvl + (w.eng != o.eng))
                    for r in rd.get(t, ()):
                        l = max(l, r.lvl + (r.eng != o.eng))
                o.lvl = l
                le[o.eng] = l
                for t, g in o.reads:
                    rd.setdefault(t, []).append(o)
                for t, g in o.writes:
                    lw[t] = o
                    rd[t] = []
                maxl = max(maxl, l)
            recs.append(ops)
        ev = []
        for i in range(n):
            st = i + (extra(i) if extra is not None else 0)
            for l in sorted(set(o.lvl for o in recs[i])):
                ev.append((st + l, -l, i))
        ev.sort()
        for (_, ml, i) in ev:
            for o in recs[i]:
                if o.lvl == -ml:
                    self.commit(o)
        return maxl

    def dma(self, eng, out, in_, reads=(), writes=(), **kw):
        return self.op(eng, lambda e: e.dma_start(out=out, in_=in_, **kw), reads, writes, dma=True)

    def barrier(self):
        lasts = set()
        for e in ENGS:
            if self.ops[e]:
                lasts.add(self.ops[e][-1])
            lasts.update(self.recent_dma[e])
        for e in ENGS:
            o = Op(e, len(self.ops[e]), None, set(lasts), False)
            self.ops[e].append(o)
        self.last_w = {}
        self.readers = {}
        self.gen = {}

    def emit(self, final_waits=()):
        nc = self.nc
        for e in ENGS:
            for o in self.ops[e]:
                for d in o.deps:
                    if d.eng == "pe" and o.eng == "pe" and not d.dma:
                        continue
                    d.needs = True
        for o in final_waits:
            o.needs = True
        sem_ctx = []
        sems = {}

        def get_sem(name):
            if name not in sems:
                cm = nc.semaphore(name)
                sems[name] = cm.__enter__()
                sem_ctx.append(cm)

        for e in ENGS:
            cnt = 0
            dcnt = 0
            for o in self.ops[e]:
                if o.dma:
                    ns = NSLOTS.get(e, NSLOT)
                    slot = dcnt % ns
                    get_sem(f"d_{e}_{slot}")
                    o.sig = (f"d_{e}_{slot}", 16 * (dcnt // ns + 1))
                    dcnt += 1
                elif o.needs and o.fn is not None:
                    ep = cnt // EPOCH
                    get_sem(f"c_{e}_{ep}")
                    o.sig = (f"c_{e}_{ep}", cnt % EPOCH + 1)
                    cnt += 1
        for e in ENGS:
            prev = None
            for o in self.ops[e]:
                if o.fn is None:
                    o.sig = prev
                elif o.sig is not None and not o.dma:
                    prev = o.sig
        handles = {"pe": "tensor", "act": "scalar", "dve": "vector", "pool": "gpsimd", "sp": "sync"}
        with nc.Block() as block:
            for e in ENGS:
                ops = self.ops[e]
                fw = list(final_waits) if e == "sp" else []

                def body(h, ops=ops, fw=fw):
                    waited = {}

                    def wait(sig):
                        if sig is None:
                            return
                        k, val = sig
                        if waited.get(k, 0) >= val:
                            return
                        waited[k] = val
                        h.wait_ge(sems[k], val)

                    for o in ops:
                        for d in sorted(o.deps, key=lambda d: (d.eng, d.idx)):
                            if d.eng == "pe" and o.eng == "pe" and not d.dma:
                                continue
                            wait(d.sig)
                        if o.fn is None:
                            continue
                        if o.dma:
                            sk, val = o.sig
                            if val > 16:
                                wait((sk, val - 16))
                            o.fn(h).then_inc(sems[sk], 16)
                        else:
                            ins = o.fn(h)
                            if o.needs:
                                ins.then_inc(sems[o.sig[0]], 1)
                    for o in fw:
                        wait(o.sig)

                if ops or fw:
                    getattr(block, handles[e])(body)
        for cm in reversed(sem_ctx):
            cm.__exit__(None, None, None)


class Ring:
    def __init__(self, alloc, name, n, shape, dt):
        self.t = [alloc(f"{name}{i}", shape, dt) for i in range(n)]
        self.name, self.n, self.i = name, n, 0

    def next(self):
        k = self.i % self.n
        g = self.i // self.n
        self.i += 1
        return self.t[k], GTag((self.name, k), g)


class SRing:
    def __init__(self, alloc, name, n, width, dt):
        self.t = alloc(name, [128, n, width], dt)
        self.name, self.n, self.i = name, n, 0

    def next(self):
        k = self.i % self.n
        g = self.i // self.n
        self.i += 1
        return self.t[:, k, :], GTag((self.name, k), g)


def pipeline(n, stages):
    ns = len(stages)
    for step in range(n + ns - 1):
        for k in reversed(range(ns)):
            i = step - k
            if 0 <= i < n:
                stages[k](i)


def build(debug=False):
    nc = bass.Bass("TRN2", target_bir_lowering=False)
    S = Sched(nc)

    def din(name, shape):
        return nc.dram_tensor(name, shape, F32, kind="ExternalInput").ap()

    xs = din("xs", [4096, 1024])
    xo = din("xo", [2048, 1024])
    csk_m = din("csk_m", [4096, 32])
    csq_m = din("csq_m", [2048, 32])
    csk_d = din("csk_d", [4096, 16])
    csq_d = din("csq_d", [2048, 16])
    masks_d = din("masks", [8, 128, 512])
    ident_d = din("ident", [128, 128])
    w_in = din("w_in", [1024, 4000])
    w_uq = din("w_uq", [256, 768])
    w_ukv = din("w_ukv", [128, 1024])
    w_mup = din("w_mup", [512, 1024])
    w_dup = din("w_dup", [512, 1024])
    w_out = din("w_out", [1024, 1024])
    w_rt = din("w_rt", [1024, 36])
    b_rt = din("b_rt", [1, 36])
    W_all = [din(f"W_all{q}", [4096, 6144 // WSPLIT]) for q in range(WSPLIT)]
    U_d = din("U_tri", [128, 128])
    ones_d = din("ones_m", [128, 128])
    thr_d = din("thr", [1, 16])
    ltm_d = din("ltm", [1, 1024])
    evec_d = din("evec", [1, 32])
    kvec_d = din("kvec", [1, 48])
    pidx_d = din("pidx", [128, 1])
    vec_names = dict(norm_mix=1024, qlat_norm=256, kvlat_norm=128, mq_gain=96, mk_gain=96, dq_gain=64,
                     dk_gain=64, lq1=64, lk1=64, lq2=64, lk2=64, subln=128, norm_ffn=1024)
    vec_d = {k: din(k, [1, n]) for k, n in vec_names.items()}
    out_d = nc.dram_tensor("out", [2048, 1024], F32, kind="ExternalOutput").ap()
    skind = "ExternalOutput" if debug else "Internal"
    xmid_d = nc.dram_tensor("xmid_scr", [2048, 1024], F32, kind=skind).ap()
    od_scr = nc.dram_tensor("od_scr", [2048, 512], BF, kind=skind).ap()
    om_scr = nc.dram_tensor("om_scr", [2048, 512], BF, kind=skind).ap()
    qm_scr = nc.dram_tensor("qm_scr", [16, 96, 1024], BF, kind="Internal").ap()
    hT_scr = nc.dram_tensor("hT_scr", [48, 128, 1024], BF, kind="Internal").ap()
    xs_scr = nc.dram_tensor("xs_scr", [NT * 256, 1024], BF, kind="Internal").ap()
    y_scr = nc.dram_tensor("y_scr", [NT * 256, 1024], F32, kind="Internal").ap()
    dbg = {}
    if debug:
        dbg["slot"] = nc.dram_tensor("dbg_slot", [128, 32], I32, kind="ExternalOutput").ap()
        dbg["widx"] = nc.dram_tensor("dbg_widx", [128, NT], I32, kind="ExternalOutput").ap()
    finals = []

    with contextlib.ExitStack() as G:
        def galloc(name, shape, dt):
            return G.enter_context(nc.sbuf_tensor("s_" + name, shape, dt))

        def ps_alloc_in(stack):
            def f(name, shape, dt):
                return stack.enter_context(nc.psum_tensor(name, shape, dt))
            return f

        cur = {}
        ident = galloc("ident", [128, 128], BF)
        gains = {k: galloc("g_" + k, [128, n], F32) for k, n in vec_names.items()}
        neg_lam = galloc("neg_lam", [128, 1], F32)
        lamt = galloc("lamt", [128, 4], F32)
        lamj = galloc("lamj", [128, 64], F32)
        xt_r = Ring(galloc, "xt", 2, [128, 1024], F32)
        sq_r = Ring(galloc, "sq", 2, [128, 1024], BF)
        st_r = SRing(galloc, "st", 96, 8, F32)
        ste_r = SRing(galloc, "ste", 4, 8, F32)
        hb_r = Ring(galloc, "hb", 2, [128, 1024], BF)
        hT_r = Ring(galloc, "hT", 3, [128, 8, 128], BF)

        def prep_rings(alloc, vfw, nvf):
            cur["vf"] = Ring(alloc, "vf", nvf, [128, vfw], F32)
            cur["vs"] = Ring(alloc, "vs", 2, [128, 768], F32)
            cur["vb"] = Ring(alloc, "vb", 6, [128, 768], BF)
            cur["rt"] = Ring(alloc, "rt", 2, [128, 4, 8, 16], F32)

        def attn_rings(alloc):
            cur["pb"] = Ring(alloc, "pb", 6, [128, 512], BF)
            cur["masks"] = alloc("masks", [128, 8, 512], BF)
            S.dma("pool", cur["masks"][:], masks_d.rearrange("m p q -> p m q"), writes=["masks"])

        S.dma("pool", ident[:], ident_d, writes=["ident"])
        for k in vec_names:
            S.dma("sp", gains[k][:], vec_d[k].partition_broadcast(128), writes=["g_" + k])

        def A(eng, method, reads, writes, **kw):
            return S.op(eng, lambda e: getattr(e, method)(**kw), reads, writes)

        def MM(out, pairs, reads, writes, start=True, stop=True):
            def fn(e):
                ins = None
                n = len(pairs)
                for i, (l, r) in enumerate(pairs):
                    ins = e.matmul(out, lhsT=l, rhs=r, start=(start and i == 0), stop=(stop and i == n - 1))
                return ins
            return S.op("pe", fn, reads, writes)

        def TR(items, reads, writes):
            def fn(e):
                ins = None
                for (o, i) in items:
                    ins = e.transpose(out=o, in_=i, identity=ident[0:i.shape[0], 0:i.shape[0]])
                return ins
            return S.op("pe", fn, list(reads) + ["ident"], writes)

        def evac(eng, reads, writes, out, in_):
            if eng == "act":
                return A("act", "copy", reads, writes, out=out, in_=in_)
            return A(eng, "tensor_copy", reads, writes, out=out, in_=in_)

        for k, (a, b) in enumerate((("lq1", "lk1"), ("lq2", "lk2"))):
            A("dve", "tensor_tensor", ["g_" + a, "g_" + b], ["lamj"], out=lamj[:], in0=gains[a][:], in1=gains[b][:], op=ALU.mult)
            A("dve", "tensor_reduce", ["lamj"], [("lamt", k)], out=lamt[:, k:k + 1], in_=lamj[:], axis=AX.X, op=ALU.add)
            A("act", "activation", [("lamt", k)], [("lamt", k)], out=lamt[:, k:k + 1], in_=lamt[:, k:k + 1], func=AF.Exp)
        A("dve", "tensor_tensor", [("lamt", 0), ("lamt", 1)], [("lamt", 2)], out=lamt[:, 2:3], in0=lamt[:, 1:2], in1=lamt[:, 0:1], op=ALU.subtract)
        A("dve", "tensor_scalar", [("lamt", 2)], ["neg_lam"], out=neg_lam[:], in0=lamt[:, 2:3], scalar1=-LAMBDA_INIT, scalar2=None, op0=ALU.add)
        A("dve", "tensor_scalar", ["g_subln"], ["g_subln"], out=gains["subln"][:], in0=gains["subln"][:], scalar1=1.0 - LAMBDA_INIT, scalar2=None, op0=ALU.mult)

        def rstd_from_ss(ss_ap, tag, D):
            if USE_POW:
                A("dve", "tensor_scalar", [tag], [tag], out=ss_ap, in0=ss_ap, scalar1=1.0 / D, scalar2=EPS, op0=ALU.mult, op1=ALU.add)
                A("dve", "tensor_scalar", [tag], [tag], out=ss_ap, in0=ss_ap, scalar1=-0.5, scalar2=None, op0=ALU.pow)
            else:
                A("act", "activation", [tag], [tag], out=ss_ap, in_=ss_ap, func=AF.Sqrt, scale=1.0 / D, bias=EPS)
                A("dve", "reciprocal", [tag], [tag], out=ss_ap, in_=ss_ap)

        def make_h(src_rows, gain_key, keep_x=None):
            if keep_x is None:
                xt, xtag = xt_r.next()
            else:
                xt, xtag = keep_x
            S.dma("sp", xt[:], src_rows, writes=[xtag])
            sq, sqtag = sq_r.next()
            st, sttag = st_r.next()
            A("act", "activation", [xtag], [sqtag, sttag], out=sq[:], in_=xt[:], func=AF.Square, accum_out=st[:, 0:1])
            rstd_from_ss(st[:, 0:1], sttag, 1024)
            hb, hbtag = hb_r.next()
            A("dve", "scalar_tensor_tensor", [xtag, sttag, "g_" + gain_key], [hbtag], out=hb[:], in0=xt[:], scalar=st[:, 0:1],
              in1=gains[gain_key][:], op0=ALU.mult, op1=ALU.mult)
            return hb, hbtag, xt, xtag

        def transpose_to(hb, hbtag, nchunk, out_ap, out_tags, width=128, eng="act", ring="pT"):
            pT, pTtag = cur[ring].next()
            items = [(pT[0:width, c * 128:(c + 1) * 128], hb[:, c * width:(c + 1) * width]) for c in range(nchunk)]
            TR(items, [hbtag], [pTtag])
            evac(eng, [pTtag], out_tags, out_ap, pT[0:width, 0:nchunk * 128].rearrange("p (c t) -> p c t", c=nchunk))

        def normrope(v, vtag, G_, D, gain_key, rope, out_bf, out_tag, gain_eng="pool"):
            n = G_ * D
            v3 = v[:, 0:n].rearrange("p (g d) -> p g d", g=G_)
            sq, sqtag = cur["vs"].next()
            st, sttag = st_r.next()
            A("dve", "tensor_tensor", [vtag], [sqtag], out=sq[:, 0:n], in0=v[:, 0:n], in1=v[:, 0:n], op=ALU.mult)
            A("dve", "tensor_reduce", [sqtag], [sttag], out=st[:, 0:G_], in_=sq[:, 0:n].rearrange("p (g d) -> p g d", g=G_), axis=AX.X, op=ALU.add)
            rstd_from_ss(st[:, 0:G_], sttag, D)
            if G_ == 1:
                assert rope is None
                A("dve", "scalar_tensor_tensor", [vtag, sttag, "g_" + gain_key], [out_tag], out=out_bf, in0=v[:, 0:n], scalar=st[:, 0:1],
                  in1=gains[gain_key][:, 0:n], op0=ALU.mult, op1=ALU.mult)
                return
            A("dve", "tensor_tensor", [vtag, sttag], [vtag], out=v3, in0=v3, in1=st[:, 0:G_].unsqueeze(2).to_broadcast([128, G_, D]), op=ALU.mult)
            A(gain_eng, "tensor_tensor", [vtag, "g_" + gain_key], [vtag], out=v3, in0=v3,
              in1=gains[gain_key][:, 0:D].unsqueeze(1).to_broadcast([128, G_, D]), op=ALU.mult)
            if rope is not None:
                r0, R, cs, cstag = rope
                hf = R // 2
                x1 = v3[:, :, r0:r0 + hf]
                x2 = v3[:, :, r0 + hf:r0 + R]
                c = cs[:, 0:hf].unsqueeze(1).to_broadcast([128, G_, hf])
                s = cs[:, hf:R].unsqueeze(1).to_broadcast([128, G_, hf])
                rt, rttag = cur["rt"].next()
                t = [rt[:, k, 0:G_, 0:hf] for k in range(4)]
                A("pool", "tensor_tensor", [vtag, cstag], [rttag], out=t[0], in0=x1, in1=c, op=ALU.mult)
                A("pool", "tensor_tensor", [vtag, cstag], [rttag], out=t[1], in0=x2, in1=s, op=ALU.mult)
                A("pool", "tensor_tensor", [vtag, cstag], [rttag], out=t[2], in0=x2, in1=c, op=ALU.mult)
                A("pool", "tensor_tensor", [vtag, cstag], [rttag], out=t[3], in0=x1, in1=s, op=ALU.mult)
                A("pool", "tensor_tensor", [rttag], [vtag], out=x1, in0=t[0], in1=t[1], op=ALU.subtract)
                A("pool", "tensor_tensor", [rttag], [vtag], out=x2, in0=t[2], in1=t[3], op=ALU.add)
            A("act", "copy", [vtag], [out_tag], out=out_bf, in_=v[:, 0:n])

        def load_w(dst, src, tag, eng="pool"):
            return S.dma(eng, dst, src, writes=[tag])

        def exp_mask(S_ps, pstag, scale, mask_idx, eng):
            pb, pbtag = cur["pb"].next()
            A("act", "activation", [pstag], [pbtag], out=pb[:], in_=S_ps, func=AF.Exp, scale=scale)
            if mask_idx is not None:
                A(eng, "tensor_tensor", [pbtag, "masks"], [pbtag], out=pb[:], in0=pb[:], in1=cur["masks"][:, mask_idx, :], op=ALU.mult)
            return pb, pbtag

        with contextlib.ExitStack() as P:
            def palloc(name, shape, dt):
                return P.enter_context(nc.sbuf_tensor("s_" + name, shape, dt))
            KdT = palloc("KdT", [128, 4, 4096], BF)
            Vd = palloc("Vd", [128, 32, 4, 129], BF)
            QdT = palloc("QdT", [128, 4, 2048], BF)
            A("pool", "memset", [], [("Vd", i) for i in range(32)], ap=Vd[:].rearrange("p a b c -> p (a b c)"), constant=1.0)

            with contextlib.ExitStack() as PP:
                def ppalloc(name, shape, dt):
                    return PP.enter_context(nc.sbuf_tensor("s_a_" + name, shape, dt))
                Wkv = ppalloc("Wd_kv", [128, 8, 1024], BF)
                Wq = ppalloc("Wd_q", [128, 8, 512], BF)
                csk = ppalloc("csk_d", [128, 32, 16], F32)
                csq = ppalloc("csq_d", [128, 16, 16], F32)
                prep_rings(ppalloc, 512, 6)
                win3 = w_in.rearrange("(c p) n -> p c n", p=128)
                load_w(Wkv[:], win3[:, :, 928:1952], "Wd_kv")
                load_w(Wq[:], win3[:, :, 416:928], "Wd_q")
                S.dma("sp", csk[:], csk_d.rearrange("(t p) r -> p t r", p=128), writes=["csk"])
                S.dma("sp", csq[:], csq_d.rearrange("(t p) r -> p t r", p=128), writes=["csq"])
                psa = ps_alloc_in(PP)
                pmm_r = Ring(psa, "a_pmm", 4, [128, 512], F32)
                cur["pT"] = Ring(psa, "a_pT", 2, [128, 1024], BF)
                cur["pT2"] = Ring(psa, "a_pT2", 2, [128, 512], BF)
                ctx = [dict() for _ in range(48)]

                def kind(i):
                    return ("kv", i) if i < 32 else ("q", i - 32)

                def stA(i):
                    k, t = kind(i)
                    src = xs[t * 128:(t + 1) * 128, :] if k == "kv" else xo[t * 128:(t + 1) * 128, :]
                    hb, hbtag, _, _ = make_h(src, "norm_mix")
                    ctx[i]["hb"] = (hb, hbtag)

                def stB(i):
                    hb, hbtag = ctx[i]["hb"]
                    hT, hTtag = hT_r.next()
                    transpose_to(hb, hbtag, 8, hT[:], [hTtag], eng="act")
                    S.dma("sp", hT_scr[i].rearrange("p (c t) -> p c t", c=8), hT[:], reads=[hTtag], writes=[("hT_scr", i)])
                    ctx[i]["hT"] = (hT, hTtag)

                def stC(i):
                    k, t = kind(i)
                    hT, hTtag = ctx[i]["hT"]
                    vf, vftag = cur["vf"].next()
                    if k == "kv":
                        p0, p0tag = pmm_r.next()
                        p1, p1tag = pmm_r.next()
                        MM(p0[:, :], [(hT[:, c, :], Wkv[:, c, 0:512]) for c in range(8)], [hTtag, "Wd_kv"], [p0tag])
                        MM(p1[:, :], [(hT[:, c, :], Wkv[:, c, 512:1024]) for c in range(8)], [hTtag, "Wd_kv"], [p1tag])
                        A("act", "copy", [p0tag], [vftag], out=vf[:, 0:512], in_=p0[:, :])
                        A("act", "copy", [p1tag], [("Vd", t)], out=Vd[:, t, :, 0:128], in_=p1[:, :].rearrange("p (h d) -> p h d", h=4))
                    else:
                        p0, p0tag = pmm_r.next()
                        MM(p0[:, :], [(hT[:, c, :], Wq[:, c, :]) for c in range(8)], [hTtag, "Wd_q"], [p0tag])
                        A("act", "copy", [p0tag], [vftag], out=vf[:, 0:512], in_=p0[:, :])
                    ctx[i]["vf"] = (vf, vftag)

                def stD(i):
                    k, t = kind(i)
                    vf, vftag = ctx[i]["vf"]
                    vb, vbtag = cur["vb"].next()
                    if k == "kv":
                        normrope(vf, vftag, 8, 64, "dk_gain", (0, 16, csk[:, t, :], "csk"), vb[:, 0:512], vbtag, gain_eng="dve")
                    else:
                        normrope(vf, vftag, 8, 64, "dq_gain", (0, 16, csq[:, t, :], "csq"), vb[:, 0:512], vbtag, gain_eng="dve")
                    ctx[i]["vb"] = (vb, vbtag)

                def stE(i):
                    k, t = kind(i)
                    vb, vbtag = ctx[i]["vb"]
                    if k == "kv":
                        transpose_to(vb, vbtag, 4, KdT[:, :, t * 128:(t + 1) * 128], [("KdT", t)], eng="act", ring="pT2")
                    else:
                        transpose_to(vb, vbtag, 4, QdT[:, :, t * 128:(t + 1) * 128], [("QdT", t)], eng="act", ring="pT2")

                def item_a(i):
                    stA(i); stB(i); stC(i); stD(i); stE(i)
                nlv = S.auto_pipeline(48, item_a, extra=None)
                if VERBOSE:
                    print("1a prep levels", nlv)
                S.barrier()

            with contextlib.ExitStack() as PA:
                def paalloc(name, shape, dt):
                    return PA.enter_context(nc.sbuf_tensor("s_a_" + name, shape, dt))
                zt = paalloc("zt", [128, 2, 1024], BF)
                S.op("pool", lambda e: e.memset(ap=zt[:].rearrange("p a b -> p (a b)"), constant=0.0), [], ["zt"])
                for k in range(NT):
                    S.dma("sp", xs_scr[256 * k:256 * (k + 1), :].rearrange("(s p) d -> p s d", p=128), zt[:], reads=["zt"], writes=[("xs_z", k)])
                attn_rings(paalloc)
                ep_r = Ring(paalloc, "ep", 4, [128, 2, 128], F32)
                eq_r = Ring(paalloc, "eq", 4, [128, 2, 128], F32)
                ods_r = Ring(paalloc, "ods", 2, [128, 4, 512], BF)
                psa = ps_alloc_in(PA)
                sc_r = Ring(psa, "a_sc", 4, [128, 512], F32)
                Ob = [psa(f"a_O{i}", [128, 512], F32) for i in range(4)]
                sc_d = 64 ** -0.5
                units = [(j, hd, kb, u) for j in range(4) for hd in range(4) for kb in range(8 * j + 8) for u in range(2)]
                pbs = {}
                odsc = {}

                def score(i):
                    j, hd, kb, u = units[i]
                    b, btag = sc_r.next()
                    qtags = [("QdT", 4 * j + s) for s in range(4)]
                    MM(b[:, :], [(KdT[64 * u:64 * u + 64, hd, kb * 128:(kb + 1) * 128], QdT[64 * u:64 * u + 64, hd, j * 512:(j + 1) * 512])],
                       [("KdT", kb)] + qtags, [btag])
                    m = kb - 8 * j if kb >= 8 * j else None
                    pbs[i] = exp_mask(b[:, :], btag, sc_d, m, "dve")

                def epilogue(j, hd):
                    if hd == 0:
                        odsc[j] = ods_r.next()
                    ods, odstag = odsc[j]
                    parts = []
                    for half in range(2):
                        O1 = Ob[half][:, 0:258].rearrange("p (s d) -> p s d", s=2)
                        O2 = Ob[2 + half][:, 0:258].rearrange("p (s d) -> p s d", s=2)
                        st, sttag = ste_r.next()
                        A("dve", "reciprocal", [("O", half)], [(sttag, 0)], out=st[:, 0:2], in_=O1[:, :, 128])
                        A("dve", "reciprocal", [("O", 2 + half)], [(sttag, 1)], out=st[:, 2:4], in_=O2[:, :, 128])
                        A("dve", "tensor_scalar", [(sttag, 1), "neg_lam"], [(sttag, 1)], out=st[:, 2:4], in0=st[:, 2:4], scalar1=neg_lam[:, 0:1], scalar2=None, op0=ALU.mult)
                        ep, eptag = ep_r.next()
                        eq, eqtag = eq_r.next()
                        A("dve", "tensor_tensor", [("O", half), (sttag, 0)], [eptag], out=ep[:], in0=O1[:, :, 0:128],
                          in1=st[:, 0:2].unsqueeze(2).to_broadcast([128, 2, 128]), op=ALU.mult)
                        A("dve", "tensor_tensor", [("O", 2 + half), (sttag, 1)], [eqtag], out=eq[:], in0=O2[:, :, 0:128],
                          in1=st[:, 2:4].unsqueeze(2).to_broadcast([128, 2, 128]), op=ALU.mult)
                        parts.append((st, sttag, ep, eptag, eq, eqtag))
                    for half in range(2):
                        st, sttag, ep, eptag, eq, eqtag = parts[half]
                        A("dve", "tensor_tensor", [eptag, eqtag], [eptag], out=ep[:], in0=ep[:], in1=eq[:], op=ALU.add)
                        A("dve", "tensor_tensor", [eptag], [eqtag], out=eq[:], in0=ep[:], in1=ep[:], op=ALU.mult)
                        A("dve", "tensor_reduce", [eqtag], [(sttag, 2)], out=st[:, 4:6], in_=eq[:], axis=AX.X, op=ALU.add)
                        rstd_from_ss(st[:, 4:6], (sttag, 2), 128)
                        A("dve", "tensor_tensor", [eptag, (sttag, 2)], [eptag], out=ep[:], in0=ep[:],
                          in1=st[:, 4:6].unsqueeze(2).to_broadcast([128, 2, 128]), op=ALU.mult)
                        t0 = 2 * half
                        A("dve", "tensor_tensor", [eptag, "g_subln"], [(odstag, hd, half)],
                          out=ods[:, t0:t0 + 2, hd * 128:(hd + 1) * 128], in0=ep[:],
                          in1=gains["subln"][:].unsqueeze(1).to_broadcast([128, 2, 128]), op=ALU.mult)
                    if hd == 3:
                        fo = S.dma("sp", od_scr[j * 512:(j + 1) * 512, :].rearrange("(s p) f -> p s f", p=128), ods[:],
                                   reads=[(odstag, h_, half) for h_ in range(4) for half in range(2)])
                        if debug:
                            finals.append(fo)

                def pv(i):
                    j, hd, kb, u = units[i]
                    pb, pbtag = pbs.pop(i)
                    for s in range(4):
                        bank = 2 * u + s // 2
                        col = (s % 2) * 129
                        MM(Ob[bank][:, col:col + 129], [(pb[:, s * 128:(s + 1) * 128], Vd[:, kb, hd, :])],
                           [pbtag, ("Vd", kb)], [("O", bank)], start=(kb == 0 and s % 2 == 0), stop=(kb == 8 * j + 7 and s % 2 == 1))
                    if kb == 8 * j + 7 and u == 1:
                        epilogue(j, hd)

                LA = 3
                for i in range(len(units) + LA):
                    if i < len(units):
                        score(i)
                    if i >= LA:
                        pv(i - LA)
                S.barrier()

        with contextlib.ExitStack() as P:
            def palloc(name, shape, dt):
                return P.enter_context(nc.sbuf_tensor("s_" + name, shape, dt))
            KmT = palloc("KmT", [96, 8, 4096], BF)
            Vm = palloc("Vm", [128, 32, 8, 65], BF)
            A("pool", "memset", [], [("Vm", i, u) for i in range(32) for u in range(2)], ap=Vm[:].rearrange("p a b c -> p (a b c)"), constant=1.0)

            with contextlib.ExitStack() as PP:
                def ppalloc(name, shape, dt):
                    return PP.enter_context(nc.sbuf_tensor("s_b_" + name, shape, dt))
                qst_r = Ring(ppalloc, "qst", 3, [96, 8, 128], BF)
                Wkv = ppalloc("Wm_kv", [128, 8, 160], BF)
                Wq = ppalloc("Wm_q", [128, 8, 256], BF)
                Wukv = ppalloc("Wukv", [128, 1024], BF)
                Wuq = ppalloc("Wuq", [128, 2, 768], BF)
                csk = ppalloc("csk_m", [128, 32, 32], F32)
                csq = ppalloc("csq_m", [128, 16, 32], F32)
                cT_r = Ring(ppalloc, "cT", 3, [128, 2, 128], BF)
                prep_rings(ppalloc, 256, 10)
                kc_r = Ring(ppalloc, "kc", 6, [128, 768], F32)
                win3 = w_in.rearrange("(c p) n -> p c n", p=128)
                load_w(Wkv[:], win3[:, :, 256:416], "Wm_kv")
                load_w(Wq[:], win3[:, :, 0:256], "Wm_q")
                load_w(Wukv[:], w_ukv, "Wukv")
                load_w(Wuq[:], w_uq.rearrange("(c p) n -> p c n", p=128), "Wuq")
                S.dma("sp", csk[:], csk_m.rearrange("(t p) r -> p t r", p=128), writes=["csk"])
                S.dma("sp", csq[:], csq_m.rearrange("(t p) r -> p t r", p=128), writes=["csq"])
                psa = ps_alloc_in(PP)
                pmmF_r = Ring(psa, "b_pmmF", 2, [128, 512], F32)
                cur["pT"] = Ring(psa, "b_pT", 2, [128, 1024], BF)
                cur["pTk"] = Ring(psa, "b_pTk", 2, [128, 1024], BF)
                pmm_r = Ring(psa, "b_pmmC", 1, [128, 256], F32)
                cur["pTc"] = Ring(psa, "b_pTc", 1, [128, 256], BF)
                ctx = [dict() for _ in range(48)]

                def kind(i):
                    return ("kv", i) if i < 32 else ("q", i - 32)

                def stA(i):
                    pass

                def stB(i):
                    hT, hTtag = hT_r.next()
                    S.dma("sp", hT[:], hT_scr[i].rearrange("p (c t) -> p c t", c=8), writes=[hTtag])
                    ctx[i]["hT"] = (hT, hTtag)

                def stC(i):
                    k, t = kind(i)
                    hT, hTtag = ctx[i]["hT"]
                    vf, vftag = cur["vf"].next()
                    p0, p0tag = pmm_r.next()
                    if k == "kv":
                        MM(p0[:, 0:160], [(hT[:, c, :], Wkv[:, c, :]) for c in range(8)], [hTtag, "Wm_kv"], [p0tag])
                        A("act", "copy", [p0tag], [vftag], out=vf[:, 0:160], in_=p0[:, 0:160])
                    else:
                        MM(p0[:, 0:256], [(hT[:, c, :], Wq[:, c, :]) for c in range(8)], [hTtag, "Wm_q"], [p0tag])
                        A("act", "copy", [p0tag], [vftag], out=vf[:, 0:256], in_=p0[:, 0:256])
                    ctx[i]["vf"] = (vf, vftag)

                def stD(i):
                    k, t = kind(i)
                    vf, vftag = ctx[i]["vf"]
                    vb, vbtag = cur["vb"].next()
                    if k == "kv":
                        normrope(vf, vftag, 1, 128, "kvlat_norm", None, vb[:, 0:128], vbtag)
                    else:
                        normrope(vf, vftag, 1, 256, "qlat_norm", None, vb[:, 0:256], vbtag)
                    ctx[i]["vb"] = (vb, vbtag)

                def stE(i):
                    k, t = kind(i)
                    vb, vbtag = ctx[i]["vb"]
                    cT, cTtag = cT_r.next()
                    if k == "kv":
                        transpose_to(vb, vbtag, 1, cT[:, 0:1, :], [cTtag], eng="act", ring="pTc")
                    else:
                        transpose_to(vb, vbtag, 2, cT[:], [cTtag], eng="act", ring="pTc")
                    ctx[i]["cT"] = (cT, cTtag)

                def stF(i):
                    k, t = kind(i)
                    cT, cTtag = ctx[i]["cT"]
                    if k == "kv":
                        vf, vftag = ctx[i]["vf"]
                        kc, kctag = kc_r.next()
                        kc3 = kc[:, 0:768].rearrange("p (h d) -> p h d", h=8)
                        ctx[i]["kc"] = (kc, kctag)
                        for u in range(2):
                            p, ptag = pmmF_r.next()
                            MM(p[:, :], [(cT[:, 0, :], Wukv[:, u * 512:(u + 1) * 512])], [cTtag, "Wukv"], [ptag])
                            kv = p[:, :].rearrange("p (h d) -> p h d", h=4)
                            A("act", "copy", [ptag], [("Vm", t, u)], out=Vm[:, t, 4 * u:4 * u + 4, 0:64], in_=kv[:, :, 64:128])
                            A("act", "copy", [ptag], [kctag], out=kc3[:, 4 * u:4 * u + 4, 0:64], in_=kv[:, :, 0:64])
                        A("act", "copy", [vftag], [kctag], out=kc3[:, :, 64:96], in_=vf[:, 128:160].unsqueeze(1).to_broadcast([128, 8, 32]))
                    else:
                        qc, qctag = kc_r.next()
                        for u in range(2):
                            p, ptag = pmmF_r.next()
                            MM(p[:, 0:384], [(cT[:, c, :], Wuq[:, c, u * 384:(u + 1) * 384]) for c in range(2)], [cTtag, "Wuq"], [ptag])
                            evac("act", [ptag], [qctag], qc[:, u * 384:(u + 1) * 384], p[:, 0:384])
                        ctx[i]["kc"] = (qc, qctag)

                def stG(i):
                    k, t = kind(i)
                    kc, kctag = ctx[i]["kc"]
                    vb2, vb2tag = cur["vb"].next()
                    if k == "kv":
                        normrope(kc, kctag, 8, 96, "mk_gain", (64, 32, csk[:, t, :], "csk"), vb2[:, 0:768], vb2tag, gain_eng="dve")
                    else:
                        normrope(kc, kctag, 8, 96, "mq_gain", (64, 32, csq[:, t, :], "csq"), vb2[:, 0:768], vb2tag, gain_eng="dve")
                    ctx[i]["vb2"] = (vb2, vb2tag)

                def stH(i):
                    k, t = kind(i)
                    vb2, vb2tag = ctx[i]["vb2"]
                    if k == "kv":
                        transpose_to(vb2, vb2tag, 8, KmT[:, :, t * 128:(t + 1) * 128], [("KmT", t)], width=96, eng="act", ring="pTk")
                    else:
                        qst, qsttag = qst_r.next()
                        transpose_to(vb2, vb2tag, 8, qst[:], [qsttag], width=96, eng="act", ring="pTk")
                        S.dma("sp", qm_scr[t].rearrange("p (h q) -> p h q", h=8), qst[:], reads=[qsttag], writes=[("qm_scr", t)])

                def item_b(i):
                    stA(i); stB(i); stC(i); stD(i); stE(i); stF(i); stG(i); stH(i)
                nlv = S.auto_pipeline(48, item_b, extra=None)
                if VERBOSE:
                    print("1b prep levels", nlv)
                S.barrier()

            with contextlib.ExitStack() as PA:
                def paalloc(name, shape, dt):
                    return PA.enter_context(nc.sbuf_tensor("s_b_" + name, shape, dt))
                attn_rings(paalloc)
                QmT_r = Ring(paalloc, "QmT", 2, [96, 8, 512], BF)
                oms_r = Ring(paalloc, "oms", 2, [128, 4, 512], BF)
                psa = ps_alloc_in(PA)
                sc_r = Ring(psa, "b_sc", 5, [128, 512], F32)
                O_r = Ring(psa, "b_O", 2, [128, 512], F32)
                sc_m = 96 ** -0.5
                units = [(j, hd, kb) for j in range(4) for hd in range(8) for kb in range(8 * j + 8)]
                pbs = {}
                Qs = {}
                omsc = {}
                Oc = {}

                def load_q(j):
                    QmT, Qtag = QmT_r.next()
                    for s in range(4):
                        S.dma("sp", QmT[:, :, s * 128:(s + 1) * 128], qm_scr[4 * j + s].rearrange("p (h q) -> p h q", h=8),
                              writes=[(Qtag, s)])
                    Qs[j] = (QmT, Qtag)

                def score(i):
                    j, hd, kb = units[i]
                    if hd == 0 and kb == 0:
                        if j == 0:
                            load_q(0)
                        if j + 1 < 4:
                            load_q(j + 1)
                    QmT, Qtag = Qs[j]
                    b, btag = sc_r.next()
                    MM(b[:, :], [(KmT[:, hd, kb * 128:(kb + 1) * 128], QmT[:, hd, :])], [("KmT", kb)] + [(Qtag, s) for s in range(4)], [btag])
                    m = kb - 8 * j if kb >= 8 * j else None
                    pbs[i] = exp_mask(b[:, :], btag, sc_m, m, "dve")

                def pv(i):
                    j, hd, kb = units[i]
                    pb, pbtag = pbs.pop(i)
                    if kb == 0:
                        Oc[(j, hd)] = O_r.next()
                    Ot, Otag = Oc[(j, hd)]
                    for s in range(4):
                        MM(Ot[:, s * 65:(s + 1) * 65], [(pb[:, s * 128:(s + 1) * 128], Vm[:, kb, hd, :])],
                           [pbtag, ("Vm", kb, 0), ("Vm", kb, 1)], [Otag], start=(kb == 0 and s == 0), stop=(kb == 8 * j + 7 and s == 3))
                    if kb == 8 * j + 7:
                        if hd == 0:
                            omsc[j] = oms_r.next()
                        oms, omstag = omsc[j]
                        O = Ot[:, 0:260].rearrange("p (s d) -> p s d", s=4)
                        st, sttag = ste_r.next()
                        A("dve", "reciprocal", [Otag], [sttag], out=st[:, 0:4], in_=O[:, :, 64])
                        A("dve", "tensor_tensor", [Otag, sttag], [(omstag, hd)],
                          out=oms[:, :, hd * 64:(hd + 1) * 64], in0=O[:, :, 0:64],
                          in1=st[:, 0:4].unsqueeze(2).to_broadcast([128, 4, 64]), op=ALU.mult)
                        if hd == 7:
                            fo = S.dma("sp", om_scr[j * 512:(j + 1) * 512, :].rearrange("(s p) f -> p s f", p=128), oms[:],
                                       reads=[(omstag, h_) for h_ in range(8)])
                            if debug:
                                finals.append(fo)

                LA = 4
                for i in range(len(units) + LA):
                    if i < len(units):
                        score(i)
                    if i >= LA:
                        pv(i - LA)
                S.barrier()

        PS2 = G.enter_context(contextlib.ExitStack())
        psa = ps_alloc_in(PS2)
        pm_r = Ring(psa, "c_pm", 3, [128, 512], F32)
        po_r = Ring(psa, "c_po", 2, [128, 512], F32)
        pr_r = Ring(psa, "c_pr", 1, [128, 64], F32)
        cur["pT"] = Ring(psa, "c_pT", 2, [128, 1024], BF)
        with contextlib.ExitStack() as Q:
            def qalloc(name, shape, dt):
                return Q.enter_context(nc.sbuf_tensor("s_" + name, shape, dt))
            h2all = qalloc("h2all", [128, 16, 1024], BF)
            M1all = qalloc("M1all", [128, 16, 32], F32)
            M2all = qalloc("M2all", [128, 16, 32], F32)
            wts = qalloc("wts", [128, 16, 2], F32)
            slot_i = qalloc("slot_i", [128, 32], I32)
            widx_i = qalloc("widx_i", [128, NT], I32)
            with contextlib.ExitStack() as P:
                def palloc(name, shape, dt):
                    return P.enter_context(nc.sbuf_tensor("s_" + name, shape, dt))
                Wg = palloc("Wg", [128, 8, 2048], BF)
                Wmu = palloc("Wmu", [128, 4, 1024], BF)
                Wdu = palloc("Wdu", [128, 4, 1024], BF)
                Wo = palloc("Wo", [128, 8, 1024], BF)
                Wr = palloc("Wr", [128, 8, 36], BF)
                brt = palloc("brt", [128, 36], F32)
                hTq = palloc("hTq", [128, 8, 512], BF)
                omT = palloc("omT", [128, 4, 512], BF)
                odT = palloc("odT", [128, 4, 512], BF)
                mT = palloc("mT", [128, 8, 512], BF)
                xk = [palloc(f"xk{i}", [128, 1024], F32) for i in range(4)]
                ok_r = Ring(palloc, "ok", 4, [128, 512], BF)
                sg_r = Ring(palloc, "sg", 3, [128, 512], F32)
                xm_r = Ring(palloc, "xm", 2, [128, 1024], F32)
                xm_r.t += xt_r.t[:2]
                xm_r.n = 4
                lg_r = Ring(palloc, "lg", 8, [128, 64], F32)
                rw_r = Ring(palloc, "rw", 4, [128, 64], F32)
                win3 = w_in.rearrange("(c p) n -> p c n", p=128)
                load_w(Wg[:, :, 0:1024], win3[:, :, 1952:2976], ("Wg", 0))
                load_w(Wg[:, :, 1024:2048], win3[:, :, 2976:4000], ("Wg", 1))
                load_w(Wmu[:], w_mup.rearrange("(c p) n -> p c n", p=128), "Wmu")
                load_w(Wdu[:], w_dup.rearrange("(c p) n -> p c n", p=128), "Wdu")
                load_w(Wo[:], w_out.rearrange("(c p) n -> p c n", p=128), "Wo")
                load_w(Wr[:], w_rt.rearrange("(c p) n -> p c n", p=128), "Wr")
                S.dma("sp", brt[:], b_rt.partition_broadcast(128), writes=["brt"])

                for j in range(4):
                    def p_item(s, j=j):
                        t = 4 * j + s
                        oks = []
                        for scr in (om_scr, od_scr):
                            ok, oktag = ok_r.next()
                            ld = S.dma("sp", ok[:], scr[t * 128:(t + 1) * 128, :], writes=[oktag])
                            oks.append((ok, oktag))
                        pT, pTtag = cur["pT"].next()
                        TR([(pT[:, q * 512 + c * 128:q * 512 + (c + 1) * 128], oks[q][0][:, c * 128:(c + 1) * 128]) for q in range(2) for c in range(4)],
                           [oks[0][1], oks[1][1]], [pTtag])
                        evac("act", [pTtag], [("omT", s)], omT[:, :, s * 128:(s + 1) * 128], pT[:, 0:512].rearrange("p (c t) -> p c t", c=4))
                        evac("act", [pTtag], [("odT", s)], odT[:, :, s * 128:(s + 1) * 128], pT[:, 512:1024].rearrange("p (c t) -> p c t", c=4))
                        hb, hbtag, _, _ = make_h(xo[t * 128:(t + 1) * 128, :], "norm_mix", keep_x=(xk[s], ("xk", s)))
                        transpose_to(hb, hbtag, 8, hTq[:, :, s * 128:(s + 1) * 128], [("hTq", s)])
                    S.auto_pipeline(4, p_item)
                    hq = [("hTq", s) for s in range(4)]
                    for m in range(8):
                        p0, p0t = pm_r.next()
                        MM(p0[:, :], [(Wg[:, c, m * 128:(m + 1) * 128], hTq[:, c, :]) for c in range(8)], hq + [("Wg", 0)], [p0t])
                        p2, p2t = pm_r.next()
                        MM(p2[:, :], [(Wmu[:, c, m * 128:(m + 1) * 128], omT[:, c, :]) for c in range(4)], [("omT", s) for s in range(4)] + ["Wmu"], [p2t])
                        s1, s1tag = sg_r.next()
                        A("act", "activation", [p0t], [s1tag], out=s1[:], in_=p0[:, :], func=AF.Sigmoid)
                        A("dve", "tensor_tensor", [s1tag, p2t], [s1tag], out=s1[:], in0=s1[:], in1=p2[:, :], op=ALU.mult)
                        p1, p1t = pm_r.next()
                        MM(p1[:, :], [(Wg[:, c, 1024 + m * 128:1024 + (m + 1) * 128], hTq[:, c, :]) for c in range(8)], hq + [("Wg", 1)], [p1t])
                        p3, p3t = pm_r.next()
                        MM(p3[:, :], [(Wdu[:, c, m * 128:(m + 1) * 128], odT[:, c, :]) for c in range(4)], [("odT", s) for s in range(4)] + ["Wdu"], [p3t])
                        s2, s2tag = sg_r.next()
                        A("act", "activation", [p1t], [s2tag], out=s2[:], in_=p1[:, :], func=AF.Sigmoid)
                        A("dve", "tensor_tensor", [s2tag, p3t], [s2tag], out=s2[:], in0=s2[:], in1=p3[:, :], op=ALU.mult)
                        A("dve", "tensor_tensor", [s1tag, s2tag], [("mT", m)], out=mT[:, m, :], in0=s1[:], in1=s2[:], op=ALU.add)
                    mtags = [("mT", m) for m in range(8)]
                    def o_item(s, j=j, mtags=mtags):
                        t = 4 * j + s
                        xm, xmtag = xm_r.next()
                        for u in range(2):
                            po, pot = po_r.next()
                            MM(po[:, :], [(mT[:, c, s * 128:(s + 1) * 128], Wo[:, c, u * 512:(u + 1) * 512]) for c in range(8)], mtags + ["Wo"], [pot])
                            A("dve", "tensor_tensor", [pot, ("xk", s)], [(xmtag, u)], out=xm[:, u * 512:(u + 1) * 512], in0=po[:, :],
                              in1=xk[s][:, u * 512:(u + 1) * 512], op=ALU.add)
                        S.dma("sp", xmid_d[t * 128:(t + 1) * 128, :], xm[:], reads=[(xmtag, 0), (xmtag, 1)], writes=[("xmid_d", t)])
                        sq, sqtag = sq_r.next()
                        st, sttag = st_r.next()
                        A("act", "activation", [(xmtag, 0), (xmtag, 1)], [sqtag, sttag], out=sq[:], in_=xm[:], func=AF.Square, accum_out=st[:, 0:1])
                        rstd_from_ss(st[:, 0:1], sttag, 1024)
                        A("dve", "scalar_tensor_tensor", [(xmtag, 0), (xmtag, 1), sttag, "g_norm_ffn"], [("h2", t)], out=h2all[:, t, :], in0=xm[:], scalar=st[:, 0:1],
                          in1=gains["norm_ffn"][:], op0=ALU.mult, op1=ALU.mult)
                        h2T, h2Ttag = hT_r.next()
                        transpose_to(h2all[:, t, :], ("h2", t), 8, h2T[:], [h2Ttag])
                        pr, prt = pr_r.next()
                        MM(pr[:, 0:36], [(h2T[:, c, :], Wr[:, c, :]) for c in range(8)], [h2Ttag, "Wr"], [prt])
                        lg, lgtag = lg_r.next()
                        rw, rwtag = rw_r.next()
                        A("dve", "tensor_tensor", [prt, "brt"], [lgtag], out=lg[:, 0:36], in0=pr[:, 0:36], in1=brt[:], op=ALU.add)
                        gl = lg[:, 0:4]
                        el = lg[:, 4:36].rearrange("p (g e) -> p g e", g=4)
                        A("dve", "tensor_reduce", [lgtag], [(rwtag, 0)], out=rw[:, 0:1], in_=gl, axis=AX.X, op=ALU.max)
                        A("dve", "tensor_scalar", [(rwtag, 0)], [(rwtag, 1)], out=rw[:, 1:2], in0=rw[:, 0:1], scalar1=-1.0, scalar2=None, op0=ALU.mult)
                        A("dve", "tensor_scalar", [lgtag, (rwtag, 0)], [(rwtag, 8)], out=rw[:, 8:12], in0=gl, scalar1=rw[:, 0:1], scalar2=None, op0=ALU.is_equal)
                        A("act", "activation", [lgtag, (rwtag, 1)], [(rwtag, 2), (rwtag, 44)], out=rw[:, 44:48], in_=gl, func=AF.Exp, bias=rw[:, 1:2], accum_out=rw[:, 2:3])
                        A("dve", "reciprocal", [(rwtag, 2)], [(rwtag, 2)], out=rw[:, 2:3], in_=rw[:, 2:3])
                        l2, l2tag = lg_r.next()
                        A("dve", "tensor_tensor", [lgtag, (rwtag, 8)], [l2tag], out=l2[:, 0:32].rearrange("p (g e) -> p g e", g=4), in0=el,
                          in1=rw[:, 8:12].unsqueeze(2).to_broadcast([128, 4, 8]), op=ALU.mult)
                        A("dve", "tensor_reduce", [l2tag], [(rwtag, 12)], out=rw[:, 12:20], in_=l2[:, 0:32].rearrange("p (g e) -> p e g", g=4), axis=AX.X, op=ALU.add)
                        A("dve", "max", [(rwtag, 12)], [(rwtag, 20)], out=rw[:, 20:28], in_=rw[:, 12:20])
                        A("dve", "tensor_scalar", [(rwtag, 12), (rwtag, 20)], [(rwtag, 28)], out=rw[:, 28:36], in0=rw[:, 12:20], scalar1=rw[:, 20:21], scalar2=None, op0=ALU.is_equal)
                        A("dve", "tensor_scalar", [(rwtag, 12), (rwtag, 20)], [(rwtag, 36)], out=rw[:, 36:44], in0=rw[:, 12:20], scalar1=rw[:, 21:22], scalar2=None, op0=ALU.is_equal)
                        for (Mall, mname, c0) in ((M1all, "M1", 28), (M2all, "M2", 36)):
                            A("dve", "tensor_tensor", [(rwtag, 8), (rwtag, c0)], [(mname, t)], out=Mall[:, t, :].rearrange("p (g e) -> p g e", g=4),
                              in0=rw[:, 8:12].unsqueeze(2).to_broadcast([128, 4, 8]), in1=rw[:, c0:c0 + 8].unsqueeze(1).to_broadcast([128, 4, 8]), op=ALU.mult)
                        A("dve", "tensor_tensor", [(rwtag, 20)], [(rwtag, 3)], out=rw[:, 3:4], in0=rw[:, 21:22], in1=rw[:, 20:21], op=ALU.subtract)
                        A("act", "activation", [(rwtag, 3)], [(rwtag, 4)], out=rw[:, 4:5], in_=rw[:, 3:4], func=AF.Exp)
                        A("dve", "tensor_scalar", [(rwtag, 4)], [(rwtag, 5)], out=rw[:, 5:6], in0=rw[:, 4:5], scalar1=1.0, scalar2=None, op0=ALU.add)
                        A("dve", "reciprocal", [(rwtag, 5)], [(rwtag, 5)], out=rw[:, 5:6], in_=rw[:, 5:6])
                        A("dve", "tensor_tensor", [(rwtag, 5), (rwtag, 2)], [("wts", t)], out=wts[:, t, 0:1], in0=rw[:, 5:6], in1=rw[:, 2:3], op=ALU.mult)
                        A("dve", "tensor_tensor", [("wts", t), (rwtag, 4)], [("wts", t)], out=wts[:, t, 1:2], in0=wts[:, t, 0:1], in1=rw[:, 4:5], op=ALU.mult)
                    S.auto_pipeline(4, o_item)
                S.barrier()
            PS2.close()

            with contextlib.ExitStack() as PB:
                pba = ps_alloc_in(PB)

                def balloc(name, shape, dt):
                    return PB.enter_context(nc.sbuf_tensor("s_r_" + name, shape, dt))
                rank_ps = pba("r_rank", [128, 512], F32)
                tot_ps = pba("r_tot", [128, 32], F32)
                Ub = balloc("U", [128, 128], BF)
                onesb = balloc("ones", [128, 128], BF)
                thr = balloc("thr", [128, 16], F32)
                ltm = balloc("ltm", [128, 32, 32], F32)
                evec = balloc("evec", [128, 32], F32)
                kvec = balloc("kvec", [128, NT], F32)
                pidx = balloc("pidx", [128, 1], F32)
                S.dma("pool", Ub[:], U_d, writes=["U"])
                S.dma("pool", onesb[:], ones_d, writes=["ones"])
                S.dma("sp", thr[:], thr_d.partition_broadcast(128), writes=["thr"])
                S.dma("sp", ltm[:].rearrange("p a b -> p (a b)"), ltm_d.partition_broadcast(128), writes=["ltm"])
                S.dma("sp", evec[:], evec_d.partition_broadcast(128), writes=["evec"])
                S.dma("sp", kvec[:], kvec_d.partition_broadcast(128), writes=["kvec"])
                S.dma("sp", pidx[:], pidx_d, writes=["pidx"])
                Mb = balloc("Mb", [128, 16, 32], BF)
                mtags = [("M1", t) for t in range(16)] + [("M2", t) for t in range(16)]
                A("dve", "tensor_tensor", mtags, ["Mb"], out=Mb[:], in0=M1all[:], in1=M2all[:], op=ALU.add)

                def rank_fn(e):
                    first = True
                    ins = None
                    for t in range(16):
                        lst = [(Ub, t)] + [(onesb, t2) for t2 in range(t)]
                        for i, (l, tt) in enumerate(lst):
                            ins = e.matmul(rank_ps[:, t * 32:(t + 1) * 32], lhsT=l[:], rhs=Mb[:, tt, :], start=first,
                                           stop=(t == 15 and i == len(lst) - 1))
                            first = False
                    return ins
                S.op("pe", rank_fn, ["Mb", "U", "ones"], ["rank_ps"])
                MM(tot_ps[:, :], [(onesb[:], Mb[:, t, :]) for t in range(16)], ["Mb", "ones"], ["tot_ps"])
                n_sb = balloc("n", [128, 32], F32)
                A("dve", "tensor_copy", ["tot_ps"], ["n"], out=n_sb[:], in_=tot_ps[:, :])
                cmp = balloc("cmp", [128, 32, 16], F32)
                A("dve", "tensor_tensor", ["n", "thr"], ["cmp"], out=cmp[:], in0=n_sb[:].unsqueeze(2).to_broadcast([128, 32, 16]),
                  in1=thr[:].unsqueeze(1).to_broadcast([128, 32, 16]), op=ALU.is_gt)
                ntl = balloc("ntl", [128, 32], F32)
                A("dve", "tensor_reduce", ["cmp"], ["ntl"], out=ntl[:], in_=cmp[:], axis=AX.X, op=ALU.add)
                tmp = balloc("tmp", [128, 32, 32], F32)
                A("dve", "tensor_tensor", ["ntl", "ltm"], ["tmp"], out=tmp[:], in0=ltm[:], in1=ntl[:].unsqueeze(1).to_broadcast([128, 32, 32]), op=ALU.mult)
                tst = balloc("tst", [128, 32], F32)
                A("dve", "tensor_reduce", ["tmp"], ["tst"], out=tst[:], in_=tmp[:], axis=AX.X, op=ALU.add)
                tend = balloc("tend", [128, 32], F32)
                A("dve", "tensor_tensor", ["tst", "ntl"], ["tend"], out=tend[:], in0=tst[:], in1=ntl[:], op=ALU.add)
                sbase = balloc("sbase", [128, 32], F32)
                A("dve", "tensor_scalar", ["tst"], ["sbase"], out=sbase[:], in0=tst[:], scalar1=256.0, scalar2=None, op0=ALU.mult)
                slotm = balloc("slotm", [128, 16, 32], F32)
                A("dve", "tensor_tensor", ["rank_ps", "sbase"], ["slotm"], out=slotm[:], in0=rank_ps[:, :].rearrange("p (t e) -> p t e", t=16),
                  in1=sbase[:].unsqueeze(1).to_broadcast([128, 16, 32]), op=ALU.add)
                prod = balloc("prod", [128, 16, 32], F32)
                slotf = balloc("slotf", [128, 2, 16], F32)
                for j, (Mall, mname) in enumerate(((M1all, "M1"), (M2all, "M2"))):
                    A("dve", "tensor_tensor", ["slotm"] + [(mname, t) for t in range(16)], ["prod"], out=prod[:], in0=slotm[:], in1=Mall[:], op=ALU.mult)
                    A("dve", "tensor_reduce", ["prod"], [("slotf", j)], out=slotf[:, j, :], in_=prod[:], axis=AX.X, op=ALU.add)
                A("dve", "tensor_copy", [("slotf", 0), ("slotf", 1)], ["slot_i"], out=slot_i[:], in_=slotf[:].rearrange("p j t -> p (j t)"))
                ind = balloc("ind", [128, NT, 32], F32)
                ind2 = balloc("ind2", [128, NT, 32], F32)
                A("dve", "tensor_tensor", ["tst", "kvec"], ["ind"], out=ind[:], in0=tst[:].unsqueeze(1).to_broadcast([128, NT, 32]),
                  in1=kvec[:].unsqueeze(2).to_broadcast([128, NT, 32]), op=ALU.is_le)
                A("dve", "tensor_tensor", ["tend", "kvec"], ["ind2"], out=ind2[:], in0=tend[:].unsqueeze(1).to_broadcast([128, NT, 32]),
                  in1=kvec[:].unsqueeze(2).to_broadcast([128, NT, 32]), op=ALU.is_gt)
                A("dve", "tensor_tensor", ["ind", "ind2"], ["ind"], out=ind[:], in0=ind[:], in1=ind2[:], op=ALU.mult)
                used = balloc("used", [128, NT], F32)
                A("dve", "tensor_reduce", ["ind"], ["used"], out=used[:], in_=ind[:], axis=AX.X, op=ALU.add)
                A("dve", "tensor_tensor", ["ind", "evec"], ["ind2"], out=ind2[:], in0=ind[:], in1=evec[:].unsqueeze(1).to_broadcast([128, NT, 32]), op=ALU.mult)
                ek = balloc("ek", [128, NT], F32)
                A("dve", "tensor_reduce", ["ind2"], ["ek"], out=ek[:], in_=ind2[:], axis=AX.X, op=ALU.add)
                A("dve", "tensor_scalar", ["ek", "pidx"], ["ek"], out=ek[:], in0=ek[:], scalar1=128.0, scalar2=pidx[:, 0:1], op0=ALU.mult, op1=ALU.add)
                A("dve", "tensor_scalar", ["ek"], ["ek"], out=ek[:], in0=ek[:], scalar1=-BIG, scalar2=None, op0=ALU.add)
                A("dve", "tensor_tensor", ["ek", "used"], ["ek"], out=ek[:], in0=ek[:], in1=used[:], op=ALU.mult)
                A("dve", "tensor_scalar", ["ek"], ["ek"], out=ek[:], in0=ek[:], scalar1=BIG, scalar2=None, op0=ALU.add)
                A("dve", "tensor_copy", ["ek"], ["widx_i"], out=widx_i[:], in_=ek[:])
                if debug:
                    finals.append(S.dma("sp", dbg["slot"], slot_i[:], reads=["slot_i"]))
                    finals.append(S.dma("sp", dbg["widx"], widx_i[:], reads=["widx_i"]))
                S.barrier()

            with contextlib.ExitStack() as P:
                def palloc(name, shape, dt):
                    return P.enter_context(nc.sbuf_tensor("s_d_" + name, shape, dt))
                psa = ps_alloc_in(P)
                Wt_r = Ring(palloc, "Wt", 6, [128, 6144], BF)
                xtok_r = Ring(palloc, "xtok", 2, [128, 2, 1024], BF)
                xT_r = Ring(palloc, "xT", 2, [128, 8, 256], BF)
                sl_r = Ring(palloc, "sl", 4, [128, 2, 256], F32)
                hid_r = Ring(palloc, "hid", 2, [128, 2, 256], BF)
                ysb_r = Ring(palloc, "ysb", 2, [128, 2, 1024], F32)
                pT3 = Ring(psa, "d_pT", 2, [128, 1024], BF)
                pgu_r = Ring(psa, "d_gu", 2, [128, 512], F32)
                psy_r = Ring(psa, "d_y", 4, [128, 512], F32)
                sc_tags = []
                bc_reg = {}

                def issue_wgather(k):
                    Wt, Wtag = Wt_r.next()
                    for q in range(WSPLIT):
                        w0, w1 = q * (6144 // WSPLIT), (q + 1) * (6144 // WSPLIT)

                        def wg(e, w0=w0, w1=w1, q=q):
                            if "r" not in bc_reg:
                                bc_reg["r"] = e.to_reg(4095)
                            return e.indirect_dma_start(out=Wt[:, w0:w1], out_offset=None, in_=W_all[q],
                                                        in_offset=bass.IndirectOffsetOnAxis(ap=widx_i[:, k:k + 1], axis=0),
                                                        bounds_check=bc_reg["r"], oob_is_err=False)
                        S.op("pool", wg, reads=["widx_i"], writes=[(Wtag, q)], dma=True)
                    return Wt, Wtag
                NPRE = 6
                pre_w = [issue_wgather(k) for k in range(NPRE)]
                for t in range(16):
                    for j in range(2):
                        col = j * 16 + t
                        tg = ("xs_scr", col)
                        sc_tags.append(tg)
                        S.op("pool", lambda e, t=t, col=col: e.indirect_dma_start(
                            out=xs_scr, out_offset=bass.IndirectOffsetOnAxis(ap=slot_i[:, col:col + 1], axis=0),
                            in_=h2all[:, t, :], in_offset=None), reads=["slot_i", ("h2", t)], writes=[tg], dma=True)

                def tile_item(k):
                    Wt, Wtag = pre_w[k] if k < NPRE else issue_wgather(k)
                    Wtags = [(Wtag, q) for q in range(WSPLIT)]
                    xtok, xtag = xtok_r.next()
                    S.dma("sp", xtok[:], xs_scr[256 * k:256 * (k + 1), :].rearrange("(s p) d -> p s d", p=128), reads=sc_tags, writes=[xtag])
                    xT, xTtag = xT_r.next()
                    for s_ in range(2):
                        pT, pTtag = pT3.next()
                        TR([(pT[:, c * 128:(c + 1) * 128], xtok[:, s_, c * 128:(c + 1) * 128]) for c in range(8)], [xtag], [pTtag])
                        evac("act" if s_ == 0 else "dve", [pTtag], [(xTtag, s_)], xT[:, :, s_ * 128:(s_ + 1) * 128],
                             pT[:, :].rearrange("p (c t) -> p c t", c=8))
                    xTt = [(xTtag, 0), (xTtag, 1)]
                    hid, hidtag = hid_r.next()
                    for f in range(2):
                        pgu, pgutag = pgu_r.next()
                        MM(pgu[:, 0:256], [(Wt[:, c * 256 + f * 128:c * 256 + (f + 1) * 128], xT[:, c, :]) for c in range(8)], Wtags + xTt, [pgutag])
                        MM(pgu[:, 256:512], [(Wt[:, 2048 + c * 256 + f * 128:2048 + c * 256 + (f + 1) * 128], xT[:, c, :]) for c in range(8)], Wtags + xTt, [pgutag])
                        sl, sltag = sl_r.next()
                        A("act", "activation", [pgutag], [sltag], out=sl[:, 0, :], in_=pgu[:, 0:256], func=AF.Silu)
                        A("act", "copy", [pgutag], [sltag], out=sl[:, 1, :], in_=pgu[:, 256:512])
                        A("dve", "tensor_tensor", [sltag], [(hidtag, f)], out=hid[:, f, :], in0=sl[:, 0, :], in1=sl[:, 1, :], op=ALU.mult)
                    ysb, ytag = ysb_r.next()
                    for sh in range(2):
                        for u in range(2):
                            py, pytag = psy_r.next()
                            MM(py[:, :], [(hid[:, f, sh * 128:(sh + 1) * 128], Wt[:, 4096 + f * 1024 + u * 512:4096 + f * 1024 + (u + 1) * 512]) for f in range(2)],
                               [(hidtag, 0), (hidtag, 1)] + Wtags, [pytag])
                            evac("act" if u == 0 else "dve", [pytag], [(ytag, sh, u)], ysb[:, sh, u * 512:(u + 1) * 512], py[:, :])
                    S.dma("sp", y_scr[256 * k:256 * (k + 1), :].rearrange("(s p) d -> p s d", p=128), ysb[:],
                          reads=[(ytag, sh, u) for sh in range(2) for u in range(2)], writes=[("y_scr", k)])
                nlv = S.auto_pipeline(NT, tile_item)
                if VERBOSE:
                    print("moe tile levels", nlv)
                S.barrier()
            with contextlib.ExitStack() as P:
                def palloc(name, shape, dt):
                    return P.enter_context(nc.sbuf_tensor("s_f_" + name, shape, dt))
                y1_r = Ring(palloc, "y1", 3, [128, 1024], F32)
                y2_r = Ring(palloc, "y2", 3, [128, 1024], F32)
                xf_r = Ring(palloc, "xf", 3, [128, 1024], F32)

                def fin_item(t):
                    y1, y1tag = y1_r.next()
                    y2, y2tag = y2_r.next()
                    xf, xftag = xf_r.next()
                    for (yy, yytag, col) in ((y1, y1tag, t), (y2, y2tag, 16 + t)):
                        S.op("pool", lambda e, yy=yy, col=col: e.indirect_dma_start(
                            out=yy[:], out_offset=None, in_=y_scr, in_offset=bass.IndirectOffsetOnAxis(ap=slot_i[:, col:col + 1], axis=0)),
                            reads=["slot_i"], writes=[yytag], dma=True)
                    S.dma("sp", xf[:], xmid_d[t * 128:(t + 1) * 128, :], writes=[xftag])
                    A("dve", "scalar_tensor_tensor", [y1tag, ("wts", t), xftag], [xftag], out=xf[:], in0=y1[:], scalar=wts[:, t, 0:1], in1=xf[:], op0=ALU.mult, op1=ALU.add)
                    A("dve", "scalar_tensor_tensor", [y2tag, ("wts", t), xftag], [xftag], out=xf[:], in0=y2[:], scalar=wts[:, t, 1:2], in1=xf[:], op0=ALU.mult, op1=ALU.add)
                    finals.append(S.dma("sp", out_d[t * 128:(t + 1) * 128, :], xf[:], reads=[xftag]))
                S.auto_pipeline(16, fin_item)
                S.barrier()
        S.emit(final_waits=finals)
    return nc


def _rope_cs(pos, rot):
    inv = (1.0 / (np.float32(500000.0) ** (np.arange(0, rot, 2, dtype=np.float32) / np.float32(rot)))).astype(np.float32)
    ang = pos.astype(np.float32)[:, None] * inv[None, :]
    return np.concatenate([np.cos(ang), np.sin(ang)], axis=1).astype(np.float32)


def _core_layout(h):
    own = np.concatenate([np.arange((2 * j + h) * 512, (2 * j + h) * 512 + 512) for j in range(4)])
    m = np.zeros((8, 128, 512), np.float32)
    for mi in range(8):
        kpos = mi * 128 + np.arange(128)
        qpos = h * 512 + np.arange(512)
        m[mi] = ((kpos[:, None] // 64) <= (qpos[None, :] // 64)).astype(np.float32)
    return own, m


def _moe_weight_rows(wg, wu, wd):
    g = wg.reshape(32, 8, 128, 256).transpose(0, 2, 1, 3).reshape(32, 128, 2048)
    u = wu.reshape(32, 8, 128, 256).transpose(0, 2, 1, 3).reshape(32, 128, 2048)
    d = wd.reshape(32, 2, 128, 1024).transpose(0, 2, 1, 3).reshape(32, 128, 2048)
    full = np.concatenate([g, u, d], axis=2).reshape(4096, 6144)
    wq = 6144 // WSPLIT
    return {f"W_all{q}": np.ascontiguousarray(full[:, q * wq:(q + 1) * wq]) for q in range(WSPLIT)}


_NC_CACHE = {}


def kernel(**inputs):
    f = lambda k: np.ascontiguousarray(np.asarray(inputs[k], dtype=np.float32))
    x = f("x")
    if "nc" not in _NC_CACHE:
        _NC_CACHE["nc"] = build(DEBUG)
    nc = _NC_CACHE["nc"]
    pos = np.arange(4096)
    shared = dict(
        ident=np.eye(128, dtype=np.float32),
        w_in=f("w_in")[0], w_uq=f("w_mla_uq")[0], w_ukv=f("w_mla_ukv")[0], w_mup=f("w_mla_up")[0], w_dup=f("w_diff_up")[0],
        w_out=f("w_out")[0],
        w_rt=np.ascontiguousarray(np.concatenate([f("w_router_group")[0], f("w_router_expert")[0]], axis=1)),
        b_rt=np.ascontiguousarray(np.concatenate([f("b_router_group")[0], f("b_router_expert")[0]])[None, :]),
        **_moe_weight_rows(f("w_expert_gate")[0], f("w_expert_up")[0], f("w_expert_down")[0]),
        U_tri=np.triu(np.ones((128, 128), np.float32), 1), ones_m=np.ones((128, 128), np.float32),
        thr=(256.0 * np.arange(16, dtype=np.float32))[None, :],
        ltm=np.tril(np.ones((32, 32), np.float32), -1).reshape(1, 1024),
        evec=np.arange(32, dtype=np.float32)[None, :], kvec=np.arange(NT, dtype=np.float32)[None, :],
        pidx=np.arange(128, dtype=np.float32)[:, None],
        norm_mix=f("norm_mix"), qlat_norm=f("mla_q_latent_norm"), kvlat_norm=f("mla_kv_latent_norm"), mq_gain=f("mla_q_gain"),
        mk_gain=f("mla_k_gain"), dq_gain=f("diff_q_gain"), dk_gain=f("diff_k_gain"), lq1=f("lambda_q1"), lk1=f("lambda_k1"),
        lq2=f("lambda_q2"), lk2=f("lambda_k2"), subln=f("diff_subln"), norm_ffn=f("norm_ffn"),
        csk_m=_rope_cs(pos, 32), csk_d=_rope_cs(pos, 16),
    )
    in_maps = []
    owns = []
    for c in range(8):
        b, h = c // 2, c % 2
        own, m = _core_layout(h)
        owns.append(own)
        d = dict(shared)
        d["xs"] = x[b]
        d["xo"] = np.ascontiguousarray(x[b][own])
        d["csq_m"] = _rope_cs(own, 32)
        d["csq_d"] = _rope_cs(own, 16)
        d["masks"] = m
        in_maps.append(d)
    if _NC_CACHE.get("prepare_only"):
        return nc, in_maps, owns
    res = run_bass_kernel_spmd(nc, in_maps, core_ids=list(range(8)))
    out = np.empty((4, 4096, 1024), np.float32)
    for c in range(8):
        out[c // 2][owns[c]] = res.results[c]["out"]
    kernel.last_results = res.results
    return out
```

```python
import contextlib
import numpy as np
import concourse.bass as bass
import concourse.mybir as mybir
from concourse.bass_utils import run_bass_kernel_spmd

F32 = mybir.dt.float32
BF = mybir.dt.bfloat16
I32 = mybir.dt.int32
AF = mybir.ActivationFunctionType
ALU = mybir.AluOpType
AX = mybir.AxisListType

ENGS = ("pe", "act", "dve", "pool", "sp")
NSLOT = 8
NSLOTS = {"sp": 16, "pool": 32}
EPOCH = 12000
EPS = 1e-6
LAMBDA_INIT = 0.2
DEBUG = False
USE_POW = False
VERBOSE = False
NT = 48
BIG = 1.0e6
WSPLIT = 1


class GTag:
    __slots__ = ("key", "gen")

    def __init__(self, key, gen):
        self.key, self.gen = key, gen


def norm_tag(t):
    if isinstance(t, GTag):
        return t.key, t.gen
    if isinstance(t, tuple):
        ks, g = [], 0
        for x in t:
            k, gg = norm_tag(x)
            ks.append(k)
            g = max(g, gg)
        return tuple(ks), g
    return t, 0


class Op:
    __slots__ = ("eng", "idx", "fn", "deps", "dma", "sig", "needs", "reads", "writes", "lvl")

    def __init__(self, eng, idx, fn, deps, dma):
        self.eng, self.idx, self.fn, self.deps, self.dma = eng, idx, fn, deps, dma
        self.sig = None
        self.needs = False
        self.lvl = 0


class Sched:
    def __init__(self, nc):
        self.nc = nc
        self.ops = {e: [] for e in ENGS}
        self.last_w = {}
        self.readers = {}
        self.recent_dma = {e: [] for e in ENGS}
        self.rec = None
        self.gen = {}

    def op(self, eng, fn, reads=(), writes=(), dma=False):
        o = Op(eng, -1, fn, set(), dma)
        o.reads = [norm_tag(t) for t in reads]
        o.writes = [norm_tag(t) for t in writes]
        if self.rec is not None:
            self.rec.append(o)
            return o
        return self.commit(o)

    def commit(self, o):
        eng = o.eng
        deps = o.deps
        for t, g in o.reads:
            cg = self.gen.get(t)
            assert cg is None or cg == g, f"ring too shallow: read of {t} gen {g} but current gen {cg}"
            w = self.last_w.get(t)
            if w is not None:
                deps.add(w)
        for t, g in o.writes:
            cg = self.gen.get(t)
            assert cg is None or cg <= g, f"ring too shallow: write of {t} gen {g} but current gen {cg}"
            self.gen[t] = g
            w = self.last_w.get(t)
            if w is not None:
                deps.add(w)
            for r in self.readers.get(t, ()):
                deps.add(r)
        deps.discard(o)
        o.idx = len(self.ops[eng])
        for t, g in o.reads:
            self.readers.setdefault(t, []).append(o)
        for t, g in o.writes:
            self.last_w[t] = o
            self.readers[t] = []
        self.ops[eng].append(o)
        if o.dma:
            rd = self.recent_dma[eng]
            rd.append(o)
            if len(rd) > NSLOTS.get(eng, NSLOT):
                rd.pop(0)
        return o

    def auto_pipeline(self, n, item_fn, extra=None):
        recs = []
        maxl = 0
        for i in range(n):
            self.rec = []
            item_fn(i)
            ops, self.rec = self.rec, None
            lw, rd, le = {}, {}, {}
            for o in ops:
                l = le.get(o.eng, 0)
                for t, g in o.reads:
                    w = lw.get(t)
                    if w is not None:
                        l = max(l, w.lvl + (w.eng != o.eng))
                for t, g in o.writes:
                    w = lw.get(t)
                    if w is not None:
                        l = max(l, w.lvl + (w.eng != o.eng))
                    for r in rd.get(t, ()):
                        l = max(l, r.lvl + (r.eng != o.eng))
                o.lvl = l
                le[o.eng] = l
                for t, g in o.reads:
                    rd.setdefault(t, []).append(o)
                for t, g in o.writes:
                    lw[t] = o
                    rd[t] = []
                maxl = max(maxl, l)
            recs.append(ops)
        ev = []
        for i in range(n):
            st = i + (extra(i) if extra is not None else 0)
            for l in sorted(set(o.lvl for o in recs[i])):
                ev.append((st + l, -l, i))
        ev.sort()
        for (_, ml, i) in ev:
            for o in recs[i]:
                if o.lvl == -ml:
                    self.commit(o)
        return maxl

    def dma(self, eng, out, in_, reads=(), writes=(), **kw):
        return self.op(eng, lambda e: e.dma_start(out=out, in_=in_, **kw), reads, writes, dma=True)

    def barrier(self):
        lasts = set()
        for e in ENGS:
            if self.ops[e]:
                lasts.add(self.ops[e][-1])
            lasts.update(self.recent_dma[e])
        for e in ENGS:
            o = Op(e, len(self.ops[e]), None, set(lasts), False)
            self.ops[e].append(o)
        self.last_w = {}
        self.readers = {}
        self.gen = {}

    def emit(self, final_waits=()):
        nc = self.nc
        for e in ENGS:
            for o in self.ops[e]:
                for d in o.deps:
                    if d.eng == "pe" and o.eng == "pe" and not d.dma:
                        continue
                    d.needs = True
        for o in final_waits:
            o.needs = True
        sem_ctx = []
        sems = {}

        def get_sem(name):
            if name not in sems:
                cm = nc.semaphore(name)
                sems[name] = cm.__enter__()
                sem_ctx.append(cm)

        for e in ENGS:
            cnt = 0
            dcnt = 0
            for o in self.ops[e]:
                if o.dma:
                    ns = NSLOTS.get(e, NSLOT)
                    slot = dcnt % ns
                    get_sem(f"d_{e}_{slot}")
                    o.sig = (f"d_{e}_{slot}", 16 * (dcnt // ns + 1))
                    dcnt += 1
                elif o.needs and o.fn is not None:
                    ep = cnt // EPOCH
                    get_sem(f"c_{e}_{ep}")
                    o.sig = (f"c_{e}_{ep}", cnt % EPOCH + 1)
                    cnt += 1
        for e in ENGS:
            prev = None
            for o in self.ops[e]:
                if o.fn is None:
                    o.sig = prev
                elif o.sig is not None and not o.dma:
                    prev = o.sig
        handles = {"pe": "tensor", "act": "scalar", "dve": "vector", "pool": "gpsimd", "sp": "sync"}
        with nc.Block() as block:
            for e in ENGS:
                ops = self.ops[e]
                fw = list(final_waits) if e == "sp" else []

                def body(h, ops=ops, fw=fw):
                    waited = {}

                    def wait(sig):
                        if sig is None:
                            return
                        k, val = sig
                        if waited.get(k, 0) >= val:
                            return
                        waited[k] = val
                        h.wait_ge(sems[k], val)

                    for o in ops:
                        for d in sorted(o.deps, key=lambda d: (d.eng, d.idx)):
                            if d.eng == "pe" and o.eng == "pe" and not d.dma:
                                continue
                            wait(d.sig)
                        if o.fn is None:
                            continue
                        if o.dma:
                            sk, val = o.sig
                            if val > 16:
                                wait((sk, val - 16))
                            o.fn(h).then_inc(sems[sk], 16)
                        else:
                            ins = o.fn(h)
                            if o.needs:
                                ins.then_inc(sems[o.sig[0]], 1)
                    for o in fw:
                        wait(o.sig)

                if ops or fw:
                    getattr(block, handles[e])(body)
        for cm in reversed(sem_ctx):
            cm.__exit__(None, None, None)


class Ring:
    def __init__(self, alloc, name, n, shape, dt):
        self.t = [alloc(f"{name}{i}", shape, dt) for i in range(n)]
        self.name, self.n, self.i = name, n, 0

    def next(self):
        k = self.i % self.n
        g = self.i // self.n
        self.i += 1
        return self.t[k], GTag((self.name, k), g)


class SRing:
    def __init__(self, alloc, name, n, width, dt):
        self.t = alloc(name, [128, n, width], dt)
        self.name, self.n, self.i = name, n, 0

    def next(self):
        k = self.i % self.n
        g = self.i // self.n
        self.i += 1
        return self.t[:, k, :], GTag((self.name, k), g)


def pipeline(n, stages):
    ns = len(stages)
    for step in range(n + ns - 1):
        for k in reversed(range(ns)):
            i = step - k
            if 0 <= i < n:
                stages[k](i)


def build(debug=False):
    nc = bass.Bass("TRN2", target_bir_lowering=False)
    S = Sched(nc)

    def din(name, shape):
        return nc.dram_tensor(name, shape, F32, kind="ExternalInput").ap()

    xs = din("xs", [4096, 1024])
    xo = din("xo", [2048, 1024])
    csk_m = din("csk_m", [4096, 32])
    csq_m = din("csq_m", [2048, 32])
    csk_d = din("csk_d", [4096, 16])
    csq_d = din("csq_d", [2048, 16])
    masks_d = din("masks", [8, 128, 512])
    ident_d = din("ident", [128, 128])
    w_in = din("w_in", [1024, 4000])
    w_uq = din("w_uq", [256, 768])
    w_ukv = din("w_ukv", [128, 1024])
    w_mup = din("w_mup", [512, 1024])
    w_dup = din("w_dup", [512, 1024])
    w_out = din("w_out", [1024, 1024])
    w_rt = din("w_rt", [1024, 36])
    b_rt = din("b_rt", [1, 36])
    W_all = [din(f"W_all{q}", [4096, 6144 // WSPLIT]) for q in range(WSPLIT)]
    U_d = din("U_tri", [128, 128])
    ones_d = din("ones_m", [128, 128])
    thr_d = din("thr", [1, 16])
    ltm_d = din("ltm", [1, 1024])
    evec_d = din("evec", [1, 32])
    kvec_d = din("kvec", [1, 48])
    pidx_d = din("pidx", [128, 1])
    vec_names = dict(norm_mix=1024, qlat_norm=256, kvlat_norm=128, mq_gain=96, mk_gain=96, dq_gain=64,
                     dk_gain=64, lq1=64, lk1=64, lq2=64, lk2=64, subln=128, norm_ffn=1024)
    vec_d = {k: din(k, [1, n]) for k, n in vec_names.items()}
    out_d = nc.dram_tensor("out", [2048, 1024], F32, kind="ExternalOutput").ap()
    skind = "ExternalOutput" if debug else "Internal"
    xmid_d = nc.dram_tensor("xmid_scr", [2048, 1024], F32, kind=skind).ap()
    od_scr = nc.dram_tensor("od_scr", [2048, 512], BF, kind=skind).ap()
    om_scr = nc.dram_tensor("om_scr", [2048, 512], BF, kind=skind).ap()
    qm_scr = nc.dram_tensor("qm_scr", [16, 96, 1024], BF, kind="Internal").ap()
    hT_scr = nc.dram_tensor("hT_scr", [48, 128, 1024], BF, kind="Internal").ap()
    xs_scr = nc.dram_tensor("xs_scr", [NT * 256, 1024], BF, kind="Internal").ap()
    y_scr = nc.dram_tensor("y_scr", [NT * 256, 1024], F32, kind="Internal").ap()
    dbg = {}
    if debug:
        dbg["slot"] = nc.dram_tensor("dbg_slot", [128, 32], I32, kind="ExternalOutput").ap()
        dbg["widx"] = nc.dram_tensor("dbg_widx", [128, NT], I32, kind="ExternalOutput").ap()
    finals = []

    with contextlib.ExitStack() as G:
        def galloc(name, shape, dt):
            return G.enter_context(nc.sbuf_tensor("s_" + name, shape, dt))

        def ps_alloc_in(stack):
            def f(name, shape, dt):
                return stack.enter_context(nc.psum_tensor(name, shape, dt))
            return f

        cur = {}
        ident = galloc("ident", [128, 128], BF)
        gains = {k: galloc("g_" + k, [128, n], F32) for k, n in vec_names.items()}
        neg_lam = galloc("neg_lam", [128, 1], F32)
        lamt = galloc("lamt", [128, 4], F32)
        lamj = galloc("lamj", [128, 64], F32)
        xt_r = Ring(galloc, "xt", 2, [128, 1024], F32)
        sq_r = Ring(galloc, "sq", 2, [128, 1024], BF)
        st_r = SRing(galloc, "st", 96, 8, F32)
        ste_r = SRing(galloc, "ste", 4, 8, F32)
        hb_r = Ring(galloc, "hb", 2, [128, 1024], BF)
        hT_r = Ring(galloc, "hT", 3, [128, 8, 128], BF)

        def prep_rings(alloc, vfw, nvf):
            cur["vf"] = Ring(alloc, "vf", nvf, [128, vfw], F32)
            cur["vs"] = Ring(alloc, "vs", 2, [128, 768], F32)
            cur["vb"] = Ring(alloc, "vb", 6, [128, 768], BF)
            cur["rt"] = Ring(alloc, "rt", 2, [128, 4, 8, 16], F32)

        def attn_rings(alloc):
            cur["pb"] = Ring(alloc, "pb", 6, [128, 512], BF)
            cur["masks"] = alloc("masks", [128, 8, 512], BF)
            S.dma("pool", cur["masks"][:], masks_d.rearrange("m p q -> p m q"), writes=["masks"])

        S.dma("pool", ident[:], ident_d, writes=["ident"])
        for k in vec_names:
            S.dma("sp", gains[k][:], vec_d[k].partition_broadcast(128), writes=["g_" + k])

        def A(eng, method, reads, writes, **kw):
            return S.op(eng, lambda e: getattr(e, method)(**kw), reads, writes)

        def MM(out, pairs, reads, writes, start=True, stop=True):
            def fn(e):
                ins = None
                n = len(pairs)
                for i, (l, r) in enumerate(pairs):
                    ins = e.matmul(out, lhsT=l, rhs=r, start=(start and i == 0), stop=(stop and i == n - 1))
                return ins
            return S.op("pe", fn, reads, writes)

        def TR(items, reads, writes):
            def fn(e):
                ins = None
                for (o, i) in items:
                    ins = e.transpose(out=o, in_=i, identity=ident[0:i.shape[0], 0:i.shape[0]])
                return ins
            return S.op("pe", fn, list(reads) + ["ident"], writes)

        def evac(eng, reads, writes, out, in_):
            if eng == "act":
                return A("act", "copy", reads, writes, out=out, in_=in_)
            return A(eng, "tensor_copy", reads, writes, out=out, in_=in_)

        for k, (a, b) in enumerate((("lq1", "lk1"), ("lq2", "lk2"))):
            A("dve", "tensor_tensor", ["g_" + a, "g_" + b], ["lamj"], out=lamj[:], in0=gains[a][:], in1=gains[b][:], op=ALU.mult)
            A("dve", "tensor_reduce", ["lamj"], [("lamt", k)], out=lamt[:, k:k + 1], in_=lamj[:], axis=AX.X, op=ALU.add)
            A("act", "activation", [("lamt", k)], [("lamt", k)], out=lamt[:, k:k + 1], in_=lamt[:, k:k + 1], func=AF.Exp)
        A("dve", "tensor_tensor", [("lamt", 0), ("lamt", 1)], [("lamt", 2)], out=lamt[:, 2:3], in0=lamt[:, 1:2], in1=lamt[:, 0:1], op=ALU.subtract)
        A("dve", "tensor_scalar", [("lamt", 2)], ["neg_lam"], out=neg_lam[:], in0=lamt[:, 2:3], scalar1=-LAMBDA_INIT, scalar2=None, op0=ALU.add)
        A("dve", "tensor_scalar", ["g_subln"], ["g_subln"], out=gains["subln"][:], in0=gains["subln"][:], scalar1=1.0 - LAMBDA_INIT, scalar2=None, op0=ALU.mult)

        def rstd_from_ss(ss_ap, tag, D):
            if USE_POW:
                A("dve", "tensor_scalar", [tag], [tag], out=ss_ap, in0=ss_ap, scalar1=1.0 / D, scalar2=EPS, op0=ALU.mult, op1=ALU.add)
                A("dve", "tensor_scalar", [tag], [tag], out=ss_ap, in0=ss_ap, scalar1=-0.5, scalar2=None, op0=ALU.pow)
            else:
                A("act", "activation", [tag], [tag], out=ss_ap, in_=ss_ap, func=AF.Sqrt, scale=1.0 / D, bias=EPS)
                A("dve", "reciprocal", [tag], [tag], out=ss_ap, in_=ss_ap)

        def make_h(src_rows, gain_key, keep_x=None):
            if keep_x is None:
                xt, xtag = xt_r.next()
            else:
                xt, xtag = keep_x
            S.dma("sp", xt[:], src_rows, writes=[xtag])
            sq, sqtag = sq_r.next()
            st, sttag = st_r.next()
            A("act", "activation", [xtag], [sqtag, sttag], out=sq[:], in_=xt[:], func=AF.Square, accum_out=st[:, 0:1])
            rstd_from_ss(st[:, 0:1], sttag, 1024)
            hb, hbtag = hb_r.next()
            A("dve", "scalar_tensor_tensor", [xtag, sttag, "g_" + gain_key], [hbtag], out=hb[:], in0=xt[:], scalar=st[:, 0:1],
              in1=gains[gain_key][:], op0=ALU.mult, op1=ALU.mult)
            return hb, hbtag, xt, xtag

        def transpose_to(hb, hbtag, nchunk, out_ap, out_tags, width=128, eng="act", ring="pT"):
            pT, pTtag = cur[ring].next()
            items = [(pT[0:width, c * 128:(c + 1) * 128], hb[:, c * width:(c + 1) * width]) for c in range(nchunk)]
            TR(items, [hbtag], [pTtag])
            evac(eng, [pTtag], out_tags, out_ap, pT[0:width, 0:nchunk * 128].rearrange("p (c t) -> p c t", c=nchunk))

        def normrope(v, vtag, G_, D, gain_key, rope, out_bf, out_tag, gain_eng="pool"):
            n = G_ * D
            v3 = v[:, 0:n].rearrange("p (g d) -> p g d", g=G_)
            sq, sqtag = cur["vs"].next()
            st, sttag = st_r.next()
            A("dve", "tensor_tensor", [vtag], [sqtag], out=sq[:, 0:n], in0=v[:, 0:n], in1=v[:, 0:n], op=ALU.mult)
            A("dve", "tensor_reduce", [sqtag], [sttag], out=st[:, 0:G_], in_=sq[:, 0:n].rearrange("p (g d) -> p g d", g=G_), axis=AX.X, op=ALU.add)
            rstd_from_ss(st[:, 0:G_], sttag, D)
            if G_ == 1:
                assert rope is None
                A("dve", "scalar_tensor_tensor", [vtag, sttag, "g_" + gain_key], [out_tag], out=out_bf, in0=v[:, 0:n], scalar=st[:, 0:1],
                  in1=gains[gain_key][:, 0:n], op0=ALU.mult, op1=ALU.mult)
                return
            A("dve", "tensor_tensor", [vtag, sttag], [vtag], out=v3, in0=v3, in1=st[:, 0:G_].unsqueeze(2).to_broadcast([128, G_, D]), op=ALU.mult)
            A(gain_eng, "tensor_tensor", [vtag, "g_" + gain_key], [vtag], out=v3, in0=v3,
              in1=gains[gain_key][:, 0:D].unsqueeze(1).to_broadcast([128, G_, D]), op=ALU.mult)
            if rope is not None:
                r0, R, cs, cstag = rope
                hf = R // 2
                x1 = v3[:, :, r0:r0 + hf]
                x2 = v3[:, :, r0 + hf:r0 + R]
                c = cs[:, 0:hf].unsqueeze(1).to_broadcast([128, G_, hf])
                s = cs[:, hf:R].unsqueeze(1).to_broadcast([128, G_, hf])
                rt, rttag = cur["rt"].next()
                t = [rt[:, k, 0:G_, 0:hf] for k in range(4)]
                A("pool", "tensor_tensor", [vtag, cstag], [rttag], out=t[0], in0=x1, in1=c, op=ALU.mult)
                A("pool", "tensor_tensor", [vtag, cstag], [rttag], out=t[1], in0=x2, in1=s, op=ALU.mult)
                A("pool", "tensor_tensor", [vtag, cstag], [rttag], out=t[2], in0=x2, in1=c, op=ALU.mult)
                A("pool", "tensor_tensor", [vtag, cstag], [rttag], out=t[3], in0=x1, in1=s, op=ALU.mult)
                A("pool", "tensor_tensor", [rttag], [vtag], out=x1, in0=t[0], in1=t[1], op=ALU.subtract)
                A("pool", "tensor_tensor", [rttag], [vtag], out=x2, in0=t[2], in1=t[3], op=ALU.add)
            A("act", "copy", [vtag], [out_tag], out=out_bf, in_=v[:, 0:n])

        def load_w(dst, src, tag, eng="pool"):
            return S.dma(eng, dst, src, writes=[tag])

        def exp_mask(S_ps, pstag, scale, mask_idx, eng):
            pb, pbtag = cur["pb"].next()
            A("act", "activation", [pstag], [pbtag], out=pb[:], in_=S_ps, func=AF.Exp, scale=scale)
            if mask_idx is not None:
                A(eng, "tensor_tensor", [pbtag, "masks"], [pbtag], out=pb[:], in0=pb[:], in1=cur["masks"][:, mask_idx, :], op=ALU.mult)
            return pb, pbtag

        with contextlib.ExitStack() as P:
            def palloc(name, shape, dt):
                return P.enter_context(nc.sbuf_tensor("s_" + name, shape, dt))
            KdT = palloc("KdT", [128, 4, 4096], BF)
            Vd = palloc("Vd", [128, 32, 4, 129], BF)
            QdT = palloc("QdT", [128, 4, 2048], BF)

            with contextlib.ExitStack() as PP:
                def ppalloc(name, shape, dt):
                    return PP.enter_context(nc.sbuf_tensor("s_a_" + name, shape, dt))
                Wkv = ppalloc("Wd_kv", [128, 8, 1024], BF)
                Wq = ppalloc("Wd_q", [128, 8, 512], BF)
                csk = ppalloc("csk_d", [128, 32, 16], F32)
                csq = ppalloc("csq_d", [128, 16, 16], F32)
                prep_rings(ppalloc, 512, 6)
                win3 = w_in.rearrange("(c p) n -> p c n", p=128)
                load_w(Wkv[:], win3[:, :, 928:1952], "Wd_kv")
                load_w(Wq[:], win3[:, :, 416:928], "Wd_q")
                S.dma("sp", csk[:], csk_d.rearrange("(t p) r -> p t r", p=128), writes=["csk"])
                S.dma("sp", csq[:], csq_d.rearrange("(t p) r -> p t r", p=128), writes=["csq"])
                A("pool", "memset", [], [("Vd", i) for i in range(32)], ap=Vd[:].rearrange("p a b c -> p (a b c)"), constant=1.0)
                psa = ps_alloc_in(PP)
                pmm_r = Ring(psa, "a_pmm", 4, [128, 512], F32)
                cur["pT"] = Ring(psa, "a_pT", 2, [128, 1024], BF)
                cur["pT2"] = Ring(psa, "a_pT2", 2, [128, 512], BF)
                ctx = [dict() for _ in range(48)]

                def kind(i):
                    return ("kv", i) if i < 32 else ("q", i - 32)

                def stA(i):
                    k, t = kind(i)
                    src = xs[t * 128:(t + 1) * 128, :] if k == "kv" else xo[t * 128:(t + 1) * 128, :]
                    hb, hbtag, _, _ = make_h(src, "norm_mix")
                    ctx[i]["hb"] = (hb, hbtag)

                def stB(i):
                    hb, hbtag = ctx[i]["hb"]
                    hT, hTtag = hT_r.next()
                    transpose_to(hb, hbtag, 8, hT[:], [hTtag], eng="act")
                    S.dma("sp", hT_scr[i].rearrange("p (c t) -> p c t", c=8), hT[:], reads=[hTtag], writes=[("hT_scr", i)])
                    ctx[i]["hT"] = (hT, hTtag)

                def stC(i):
                    k, t = kind(i)
                    hT, hTtag = ctx[i]["hT"]
                    vf, vftag = cur["vf"].next()
                    if k == "kv":
                        p0, p0tag = pmm_r.next()
                        p1, p1tag = pmm_r.next()
                        MM(p0[:, :], [(hT[:, c, :], Wkv[:, c, 0:512]) for c in range(8)], [hTtag, "Wd_kv"], [p0tag])
                        MM(p1[:, :], [(hT[:, c, :], Wkv[:, c, 512:1024]) for c in range(8)], [hTtag, "Wd_kv"], [p1tag])
                        A("act", "copy", [p0tag], [vftag], out=vf[:, 0:512], in_=p0[:, :])
                        A("act", "copy", [p1tag], [("Vd", t)], out=Vd[:, t, :, 0:128], in_=p1[:, :].rearrange("p (h d) -> p h d", h=4))
                    else:
                        p0, p0tag = pmm_r.next()
                        MM(p0[:, :], [(hT[:, c, :], Wq[:, c, :]) for c in range(8)], [hTtag, "Wd_q"], [p0tag])
                        A("act", "copy", [p0tag], [vftag], out=vf[:, 0:512], in_=p0[:, :])
                    ctx[i]["vf"] = (vf, vftag)

                def stD(i):
                    k, t = kind(i)
                    vf, vftag = ctx[i]["vf"]
                    vb, vbtag = cur["vb"].next()
                    if k == "kv":
                        normrope(vf, vftag, 8, 64, "dk_gain", (0, 16, csk[:, t, :], "csk"), vb[:, 0:512], vbtag, gain_eng="dve")
                    else:
                        normrope(vf, vftag, 8, 64, "dq_gain", (0, 16, csq[:, t, :], "csq"), vb[:, 0:512], vbtag, gain_eng="dve")
                    ctx[i]["vb"] = (vb, vbtag)

                def stE(i):
                    k, t = kind(i)
                    vb, vbtag = ctx[i]["vb"]
                    if k == "kv":
                        transpose_to(vb, vbtag, 4, KdT[:, :, t * 128:(t + 1) * 128], [("KdT", t)], eng="act", ring="pT2")
                    else:
                        transpose_to(vb, vbtag, 4, QdT[:, :, t * 128:(t + 1) * 128], [("QdT", t)], eng="act", ring="pT2")

                def item_a(i):
                    stA(i); stB(i); stC(i); stD(i); stE(i)
                nlv = S.auto_pipeline(48, item_a, extra=None)
                if VERBOSE:
                    print("1a prep levels", nlv)
                S.barrier()

            with contextlib.ExitStack() as PA:
                def paalloc(name, shape, dt):
                    return PA.enter_context(nc.sbuf_tensor("s_a_" + name, shape, dt))
                zt = paalloc("zt", [128, 2, 1024], BF)
                S.op("pool", lambda e: e.memset(ap=zt[:].rearrange("p a b -> p (a b)"), constant=0.0), [], ["zt"])
                for k in range(NT):
                    S.dma("sp", xs_scr[256 * k:256 * (k + 1), :].rearrange("(s p) d -> p s d", p=128), zt[:], reads=["zt"], writes=[("xs_z", k)])
                attn_rings(paalloc)
                ep_r = Ring(paalloc, "ep", 4, [128, 2, 128], F32)
                eq_r = Ring(paalloc, "eq", 4, [128, 2, 128], F32)
                ods_r = Ring(paalloc, "ods", 2, [128, 4, 512], BF)
                psa = ps_alloc_in(PA)
                sc_r = Ring(psa, "a_sc", 4, [128, 512], F32)
                Ob = [psa(f"a_O{i}", [128, 512], F32) for i in range(4)]
                sc_d = 64 ** -0.5
                units = [(j, hd, kb, u) for j in range(4) for hd in range(4) for kb in range(8 * j + 8) for u in range(2)]
                pbs = {}
                odsc = {}

                def score(i):
                    j, hd, kb, u = units[i]
                    b, btag = sc_r.next()
                    qtags = [("QdT", 4 * j + s) for s in range(4)]
                    MM(b[:, :], [(KdT[64 * u:64 * u + 64, hd, kb * 128:(kb + 1) * 128], QdT[64 * u:64 * u + 64, hd, j * 512:(j + 1) * 512])],
                       [("KdT", kb)] + qtags, [btag])
                    m = kb - 8 * j if kb >= 8 * j else None
                    pbs[i] = exp_mask(b[:, :], btag, sc_d, m, "dve")

                def epilogue(j, hd):
                    if hd == 0:
                        odsc[j] = ods_r.next()
                    ods, odstag = odsc[j]
                    parts = []
                    for half in range(2):
                        O1 = Ob[half][:, 0:258].rearrange("p (s d) -> p s d", s=2)
                        O2 = Ob[2 + half][:, 0:258].rearrange("p (s d) -> p s d", s=2)
                        st, sttag = ste_r.next()
                        A("dve", "reciprocal", [("O", half)], [(sttag, 0)], out=st[:, 0:2], in_=O1[:, :, 128])
                        A("dve", "reciprocal", [("O", 2 + half)], [(sttag, 1)], out=st[:, 2:4], in_=O2[:, :, 128])
                        A("dve", "tensor_scalar", [(sttag, 1), "neg_lam"], [(sttag, 1)], out=st[:, 2:4], in0=st[:, 2:4], scalar1=neg_lam[:, 0:1], scalar2=None, op0=ALU.mult)
                        ep, eptag = ep_r.next()
                        eq, eqtag = eq_r.next()
                        A("dve", "tensor_tensor", [("O", half), (sttag, 0)], [eptag], out=ep[:], in0=O1[:, :, 0:128],
                          in1=st[:, 0:2].unsqueeze(2).to_broadcast([128, 2, 128]), op=ALU.mult)
                        A("dve", "tensor_tensor", [("O", 2 + half), (sttag, 1)], [eqtag], out=eq[:], in0=O2[:, :, 0:128],
                          in1=st[:, 2:4].unsqueeze(2).to_broadcast([128, 2, 128]), op=ALU.mult)
                        parts.append((st, sttag, ep, eptag, eq, eqtag))
                    for half in range(2):
                        st, sttag, ep, eptag, eq, eqtag = parts[half]
                        A("pool", "tensor_tensor", [eptag, eqtag], [eptag], out=ep[:], in0=ep[:], in1=eq[:], op=ALU.add)
                        A("pool", "tensor_tensor", [eptag], [eqtag], out=eq[:], in0=ep[:], in1=ep[:], op=ALU.mult)
                        A("dve", "tensor_reduce", [eqtag], [(sttag, 2)], out=st[:, 4:6], in_=eq[:], axis=AX.X, op=ALU.add)
                        rstd_from_ss(st[:, 4:6], (sttag, 2), 128)
                        A("dve", "tensor_tensor", [eptag, (sttag, 2)], [eptag], out=ep[:], in0=ep[:],
                          in1=st[:, 4:6].unsqueeze(2).to_broadcast([128, 2, 128]), op=ALU.mult)
                        t0 = 2 * half
                        A("pool", "tensor_tensor", [eptag, "g_subln"], [(odstag, hd, half)],
                          out=ods[:, t0:t0 + 2, hd * 128:(hd + 1) * 128], in0=ep[:],
                          in1=gains["subln"][:].unsqueeze(1).to_broadcast([128, 2, 128]), op=ALU.mult)
                    if hd == 3:
                        fo = S.dma("sp", od_scr[j * 512:(j + 1) * 512, :].rearrange("(s p) f -> p s f", p=128), ods[:],
                                   reads=[(odstag, h_, half) for h_ in range(4) for half in range(2)])
                        if debug:
                            finals.append(fo)

                def pv(i):
                    j, hd, kb, u = units[i]
                    pb, pbtag = pbs.pop(i)
                    for s in range(4):
                        bank = 2 * u + s // 2
                        col = (s % 2) * 129
                        MM(Ob[bank][:, col:col + 129], [(pb[:, s * 128:(s + 1) * 128], Vd[:, kb, hd, :])],
                           [pbtag, ("Vd", kb)], [("O", bank)], start=(kb == 0 and s % 2 == 0), stop=(kb == 8 * j + 7 and s % 2 == 1))
                    if kb == 8 * j + 7 and u == 1:
                        epilogue(j, hd)

                LA = 3
                for i in range(len(units) + LA):
                    if i < len(units):
                        score(i)
                    if i >= LA:
                        pv(i - LA)
                S.barrier()

        with contextlib.ExitStack() as P:
            def palloc(name, shape, dt):
                return P.enter_context(nc.sbuf_tensor("s_" + name, shape, dt))
            KmT = palloc("KmT", [96, 8, 4096], BF)
            Vm = palloc("Vm", [128, 32, 8, 65], BF)

            with contextlib.ExitStack() as PP:
                def ppalloc(name, shape, dt):
                    return PP.enter_context(nc.sbuf_tensor("s_b_" + name, shape, dt))
                qst_r = Ring(ppalloc, "qst", 3, [96, 8, 128], BF)
                Wkv = ppalloc("Wm_kv", [128, 8, 160], BF)
                Wq = ppalloc("Wm_q", [128, 8, 256], BF)
                Wukv = ppalloc("Wukv", [128, 1024], BF)
                Wuq = ppalloc("Wuq", [128, 2, 768], BF)
                csk = ppalloc("csk_m", [128, 32, 32], F32)
                csq = ppalloc("csq_m", [128, 16, 32], F32)
                cT_r = Ring(ppalloc, "cT", 3, [128, 2, 128], BF)
                prep_rings(ppalloc, 256, 10)
                kc_r = Ring(ppalloc, "kc", 6, [128, 768], F32)
                win3 = w_in.rearrange("(c p) n -> p c n", p=128)
                load_w(Wkv[:], win3[:, :, 256:416], "Wm_kv")
                load_w(Wq[:], win3[:, :, 0:256], "Wm_q")
                load_w(Wukv[:], w_ukv, "Wukv")
                load_w(Wuq[:], w_uq.rearrange("(c p) n -> p c n", p=128), "Wuq")
                S.dma("sp", csk[:], csk_m.rearrange("(t p) r -> p t r", p=128), writes=["csk"])
                S.dma("sp", csq[:], csq_m.rearrange("(t p) r -> p t r", p=128), writes=["csq"])
                A("pool", "memset", [], [("Vm", i, u) for i in range(32) for u in range(2)], ap=Vm[:].rearrange("p a b c -> p (a b c)"), constant=1.0)
                psa = ps_alloc_in(PP)
                pmmF_r = Ring(psa, "b_pmmF", 2, [128, 512], F32)
                cur["pT"] = Ring(psa, "b_pT", 2, [128, 1024], BF)
                cur["pTk"] = Ring(psa, "b_pTk", 2, [128, 1024], BF)
                pmm_r = Ring(psa, "b_pmmC", 1, [128, 256], F32)
                cur["pTc"] = Ring(psa, "b_pTc", 1, [128, 256], BF)
                ctx = [dict() for _ in range(48)]

                def kind(i):
                    return ("kv", i) if i < 32 else ("q", i - 32)

                def stA(i):
                    pass

                def stB(i):
                    hT, hTtag = hT_r.next()
                    S.dma("sp", hT[:], hT_scr[i].rearrange("p (c t) -> p c t", c=8), writes=[hTtag])
                    ctx[i]["hT"] = (hT, hTtag)

                def stC(i):
                    k, t = kind(i)
                    hT, hTtag = ctx[i]["hT"]
                    vf, vftag = cur["vf"].next()
                    p0, p0tag = pmm_r.next()
                    if k == "kv":
                        MM(p0[:, 0:160], [(hT[:, c, :], Wkv[:, c, :]) for c in range(8)], [hTtag, "Wm_kv"], [p0tag])
                        A("act", "copy", [p0tag], [vftag], out=vf[:, 0:160], in_=p0[:, 0:160])
                    else:
                        MM(p0[:, 0:256], [(hT[:, c, :], Wq[:, c, :]) for c in range(8)], [hTtag, "Wm_q"], [p0tag])
                        A("act", "copy", [p0tag], [vftag], out=vf[:, 0:256], in_=p0[:, 0:256])
                    ctx[i]["vf"] = (vf, vftag)

                def stD(i):
                    k, t = kind(i)
                    vf, vftag = ctx[i]["vf"]
                    vb, vbtag = cur["vb"].next()
                    if k == "kv":
                        normrope(vf, vftag, 1, 128, "kvlat_norm", None, vb[:, 0:128], vbtag)
                    else:
                        normrope(vf, vftag, 1, 256, "qlat_norm", None, vb[:, 0:256], vbtag)
                    ctx[i]["vb"] = (vb, vbtag)

                def stE(i):
                    k, t = kind(i)
                    vb, vbtag = ctx[i]["vb"]
                    cT, cTtag = cT_r.next()
                    if k == "kv":
                        transpose_to(vb, vbtag, 1, cT[:, 0:1, :], [cTtag], eng="act", ring="pTc")
                    else:
                        transpose_to(vb, vbtag, 2, cT[:], [cTtag], eng="act", ring="pTc")
                    ctx[i]["cT"] = (cT, cTtag)

                def stF(i):
                    k, t = kind(i)
                    cT, cTtag = ctx[i]["cT"]
                    if k == "kv":
                        vf, vftag = ctx[i]["vf"]
                        kc, kctag = kc_r.next()
                        kc3 = kc[:, 0:768].rearrange("p (h d) -> p h d", h=8)
                        ctx[i]["kc"] = (kc, kctag)
                        for u in range(2):
                            p, ptag = pmmF_r.next()
                            MM(p[:, :], [(cT[:, 0, :], Wukv[:, u * 512:(u + 1) * 512])], [cTtag, "Wukv"], [ptag])
                            kv = p[:, :].rearrange("p (h d) -> p h d", h=4)
                            A("act", "copy", [ptag], [("Vm", t, u)], out=Vm[:, t, 4 * u:4 * u + 4, 0:64], in_=kv[:, :, 64:128])
                            A("act", "copy", [ptag], [kctag], out=kc3[:, 4 * u:4 * u + 4, 0:64], in_=kv[:, :, 0:64])
                        A("act", "copy", [vftag], [kctag], out=kc3[:, :, 64:96], in_=vf[:, 128:160].unsqueeze(1).to_broadcast([128, 8, 32]))
                    else:
                        qc, qctag = kc_r.next()
                        for u in range(2):
                            p, ptag = pmmF_r.next()
                            MM(p[:, 0:384], [(cT[:, c, :], Wuq[:, c, u * 384:(u + 1) * 384]) for c in range(2)], [cTtag, "Wuq"], [ptag])
                            evac("act", [ptag], [qctag], qc[:, u * 384:(u + 1) * 384], p[:, 0:384])
                        ctx[i]["kc"] = (qc, qctag)

                def stG(i):
                    k, t = kind(i)
                    kc, kctag = ctx[i]["kc"]
                    vb2, vb2tag = cur["vb"].next()
                    if k == "kv":
                        normrope(kc, kctag, 8, 96, "mk_gain", (64, 32, csk[:, t, :], "csk"), vb2[:, 0:768], vb2tag, gain_eng="dve")
                    else:
                        normrope(kc, kctag, 8, 96, "mq_gain", (64, 32, csq[:, t, :], "csq"), vb2[:, 0:768], vb2tag, gain_eng="dve")
                    ctx[i]["vb2"] = (vb2, vb2tag)

                def stH(i):
                    k, t = kind(i)
                    vb2, vb2tag = ctx[i]["vb2"]
                    if k == "kv":
                        transpose_to(vb2, vb2tag, 8, KmT[:, :, t * 128:(t + 1) * 128], [("KmT", t)], width=96, eng="act", ring="pTk")
                    else:
                        qst, qsttag = qst_r.next()
                        transpose_to(vb2, vb2tag, 8, qst[:], [qsttag], width=96, eng="act", ring="pTk")
                        S.dma("sp", qm_scr[t].rearrange("p (h q) -> p h q", h=8), qst[:], reads=[qsttag], writes=[("qm_scr", t)])

                def item_b(i):
                    stA(i); stB(i); stC(i); stD(i); stE(i); stF(i); stG(i); stH(i)
                nlv = S.auto_pipeline(48, item_b, extra=None)
                if VERBOSE:
                    print("1b prep levels", nlv)
                S.barrier()

            with contextlib.ExitStack() as PA:
                def paalloc(name, shape, dt):
                    return PA.enter_context(nc.sbuf_tensor("s_b_" + name, shape, dt))
                attn_rings(paalloc)
                QmT_r = Ring(paalloc, "QmT", 2, [96, 8, 512], BF)
                oms_r = Ring(paalloc, "oms", 2, [128, 4, 512], BF)
                psa = ps_alloc_in(PA)
                sc_r = Ring(psa, "b_sc", 5, [128, 512], F32)
                O_r = Ring(psa, "b_O", 2, [128, 512], F32)
                sc_m = 96 ** -0.5
                units = [(j, hd, kb) for j in range(4) for hd in range(8) for kb in range(8 * j + 8)]
                pbs = {}
                Qs = {}
                omsc = {}
                Oc = {}

                def load_q(j):
                    QmT, Qtag = QmT_r.next()
                    for s in range(4):
                        S.dma("sp", QmT[:, :, s * 128:(s + 1) * 128], qm_scr[4 * j + s].rearrange("p (h q) -> p h q", h=8),
                              writes=[(Qtag, s)])
                    Qs[j] = (QmT, Qtag)

                def score(i):
                    j, hd, kb = units[i]
                    if hd == 0 and kb == 0:
                        if j == 0:
                            load_q(0)
                        if j + 1 < 4:
                            load_q(j + 1)
                    QmT, Qtag = Qs[j]
                    b, btag = sc_r.next()
                    MM(b[:, :], [(KmT[:, hd, kb * 128:(kb + 1) * 128], QmT[:, hd, :])], [("KmT", kb)] + [(Qtag, s) for s in range(4)], [btag])
                    m = kb - 8 * j if kb >= 8 * j else None
                    pbs[i] = exp_mask(b[:, :], btag, sc_m, m, "dve")

                def pv(i):
                    j, hd, kb = units[i]
                    pb, pbtag = pbs.pop(i)
                    if kb == 0:
                        Oc[(j, hd)] = O_r.next()
                    Ot, Otag = Oc[(j, hd)]
                    for s in range(4):
                        MM(Ot[:, s * 65:(s + 1) * 65], [(pb[:, s * 128:(s + 1) * 128], Vm[:, kb, hd, :])],
                           [pbtag, ("Vm", kb, 0), ("Vm", kb, 1)], [Otag], start=(kb == 0 and s == 0), stop=(kb == 8 * j + 7 and s == 3))
                    if kb == 8 * j + 7:
                        if hd == 0:
                            omsc[j] = oms_r.next()
                        oms, omstag = omsc[j]
                        O = Ot[:, 0:260].rearrange("p (s d) -> p s d", s=4)
                        st, sttag = ste_r.next()
                        A("dve", "reciprocal", [Otag], [sttag], out=st[:, 0:4], in_=O[:, :, 64])
                        A("dve", "tensor_tensor", [Otag, sttag], [(omstag, hd)],
                          out=oms[:, :, hd * 64:(hd + 1) * 64], in0=O[:, :, 0:64],
                          in1=st[:, 0:4].unsqueeze(2).to_broadcast([128, 4, 64]), op=ALU.mult)
                        if hd == 7:
                            fo = S.dma("sp", om_scr[j * 512:(j + 1) * 512, :].rearrange("(s p) f -> p s f", p=128), oms[:],
                                       reads=[(omstag, h_) for h_ in range(8)])
                            if debug:
                                finals.append(fo)

                LA = 4
                for i in range(len(units) + LA):
                    if i < len(units):
                        score(i)
                    if i >= LA:
                        pv(i - LA)
                S.barrier()

        PS2 = G.enter_context(contextlib.ExitStack())
        psa = ps_alloc_in(PS2)
        pm_r = Ring(psa, "c_pm", 3, [128, 512], F32)
        po_r = Ring(psa, "c_po", 2, [128, 512], F32)
        pr_r = Ring(psa, "c_pr", 1, [128, 64], F32)
        cur["pT"] = Ring(psa, "c_pT", 2, [128, 1024], BF)
        with contextlib.ExitStack() as Q:
            def qalloc(name, shape, dt):
                return Q.enter_context(nc.sbuf_tensor("s_" + name, shape, dt))
            h2all = qalloc("h2all", [128, 16, 1024], BF)
            M1all = qalloc("M1all", [128, 16, 32], F32)
            M2all = qalloc("M2all", [128, 16, 32], F32)
            wts = qalloc("wts", [128, 16, 2], F32)
            slot_i = qalloc("slot_i", [128, 32], I32)
            widx_i = qalloc("widx_i", [128, NT], I32)
            with contextlib.ExitStack() as P:
                def palloc(name, shape, dt):
                    return P.enter_context(nc.sbuf_tensor("s_" + name, shape, dt))
                Wg = palloc("Wg", [128, 8, 2048], BF)
                Wmu = palloc("Wmu", [128, 4, 1024], BF)
                Wdu = palloc("Wdu", [128, 4, 1024], BF)
                Wo = palloc("Wo", [128, 8, 1024], BF)
                Wr = palloc("Wr", [128, 8, 36], BF)
                brt = palloc("brt", [128, 36], F32)
                hTq = palloc("hTq", [128, 8, 512], BF)
                omT = palloc("omT", [128, 4, 512], BF)
                odT = palloc("odT", [128, 4, 512], BF)
                mT = palloc("mT", [128, 8, 512], BF)
                xk = [palloc(f"xk{i}", [128, 1024], F32) for i in range(4)]
                ok_r = Ring(palloc, "ok", 4, [128, 512], BF)
                sg_r = Ring(palloc, "sg", 3, [128, 512], F32)
                xm_r = Ring(palloc, "xm", 2, [128, 1024], F32)
                xm_r.t += xt_r.t[:2]
                xm_r.n = 4
                lg_r = Ring(palloc, "lg", 8, [128, 64], F32)
                rw_r = Ring(palloc, "rw", 4, [128, 64], F32)
                win3 = w_in.rearrange("(c p) n -> p c n", p=128)
                load_w(Wg[:, :, 0:1024], win3[:, :, 1952:2976], ("Wg", 0))
                load_w(Wg[:, :, 1024:2048], win3[:, :, 2976:4000], ("Wg", 1))
                load_w(Wmu[:], w_mup.rearrange("(c p) n -> p c n", p=128), "Wmu")
                load_w(Wdu[:], w_dup.rearrange("(c p) n -> p c n", p=128), "Wdu")
                load_w(Wo[:], w_out.rearrange("(c p) n -> p c n", p=128), "Wo")
                load_w(Wr[:], w_rt.rearrange("(c p) n -> p c n", p=128), "Wr")
                S.dma("sp", brt[:], b_rt.partition_broadcast(128), writes=["brt"])

                for j in range(4):
                    def p_item(s, j=j):
                        t = 4 * j + s
                        oks = []
                        for scr in (om_scr, od_scr):
                            ok, oktag = ok_r.next()
                            ld = S.dma("sp", ok[:], scr[t * 128:(t + 1) * 128, :], writes=[oktag])
                            oks.append((ok, oktag))
                        pT, pTtag = cur["pT"].next()
                        TR([(pT[:, q * 512 + c * 128:q * 512 + (c + 1) * 128], oks[q][0][:, c * 128:(c + 1) * 128]) for q in range(2) for c in range(4)],
                           [oks[0][1], oks[1][1]], [pTtag])
                        evac("act", [pTtag], [("omT", s)], omT[:, :, s * 128:(s + 1) * 128], pT[:, 0:512].rearrange("p (c t) -> p c t", c=4))
                        evac("act", [pTtag], [("odT", s)], odT[:, :, s * 128:(s + 1) * 128], pT[:, 512:1024].rearrange("p (c t) -> p c t", c=4))
                        hb, hbtag, _, _ = make_h(xo[t * 128:(t + 1) * 128, :], "norm_mix", keep_x=(xk[s], ("xk", s)))
                        transpose_to(hb, hbtag, 8, hTq[:, :, s * 128:(s + 1) * 128], [("hTq", s)])
                    S.auto_pipeline(4, p_item)
                    hq = [("hTq", s) for s in range(4)]
                    for m in range(8):
                        p0, p0t = pm_r.next()
                        MM(p0[:, :], [(Wg[:, c, m * 128:(m + 1) * 128], hTq[:, c, :]) for c in range(8)], hq + [("Wg", 0)], [p0t])
                        p2, p2t = pm_r.next()
                        MM(p2[:, :], [(Wmu[:, c, m * 128:(m + 1) * 128], omT[:, c, :]) for c in range(4)], [("omT", s) for s in range(4)] + ["Wmu"], [p2t])
                        s1, s1tag = sg_r.next()
                        A("act", "activation", [p0t], [s1tag], out=s1[:], in_=p0[:, :], func=AF.Sigmoid)
                        A("dve", "tensor_tensor", [s1tag, p2t], [s1tag], out=s1[:], in0=s1[:], in1=p2[:, :], op=ALU.mult)
                        p1, p1t = pm_r.next()
                        MM(p1[:, :], [(Wg[:, c, 1024 + m * 128:1024 + (m + 1) * 128], hTq[:, c, :]) for c in range(8)], hq + [("Wg", 1)], [p1t])
                        p3, p3t = pm_r.next()
                        MM(p3[:, :], [(Wdu[:, c, m * 128:(m + 1) * 128], odT[:, c, :]) for c in range(4)], [("odT", s) for s in range(4)] + ["Wdu"], [p3t])
                        s2, s2tag = sg_r.next()
                        A("act", "activation", [p1t], [s2tag], out=s2[:], in_=p1[:, :], func=AF.Sigmoid)
                        A("dve", "tensor_tensor", [s2tag, p3t], [s2tag], out=s2[:], in0=s2[:], in1=p3[:, :], op=ALU.mult)
                        A("dve", "tensor_tensor", [s1tag, s2tag], [("mT", m)], out=mT[:, m, :], in0=s1[:], in1=s2[:], op=ALU.add)
                    mtags = [("mT", m) for m in range(8)]
                    def o_item(s, j=j, mtags=mtags):
                        t = 4 * j + s
                        xm, xmtag = xm_r.next()
                        for u in range(2):
                            po, pot = po_r.next()
                            MM(po[:, :], [(mT[:, c, s * 128:(s + 1) * 128], Wo[:, c, u * 512:(u + 1) * 512]) for c in range(8)], mtags + ["Wo"], [pot])
                            A("dve", "tensor_tensor", [pot, ("xk", s)], [(xmtag, u)], out=xm[:, u * 512:(u + 1) * 512], in0=po[:, :],
                              in1=xk[s][:, u * 512:(u + 1) * 512], op=ALU.add)
                        S.dma("sp", xmid_d[t * 128:(t + 1) * 128, :], xm[:], reads=[(xmtag, 0), (xmtag, 1)], writes=[("xmid_d", t)])
                        sq, sqtag = sq_r.next()
                        st, sttag = st_r.next()
                        A("act", "activation", [(xmtag, 0), (xmtag, 1)], [sqtag, sttag], out=sq[:], in_=xm[:], func=AF.Square, accum_out=st[:, 0:1])
                        rstd_from_ss(st[:, 0:1], sttag, 1024)
                        A("dve", "scalar_tensor_tensor", [(xmtag, 0), (xmtag, 1), sttag, "g_norm_ffn"], [("h2", t)], out=h2all[:, t, :], in0=xm[:], scalar=st[:, 0:1],
                          in1=gains["norm_ffn"][:], op0=ALU.mult, op1=ALU.mult)
                        h2T, h2Ttag = hT_r.next()
                        transpose_to(h2all[:, t, :], ("h2", t), 8, h2T[:], [h2Ttag])
                        pr, prt = pr_r.next()
                        MM(pr[:, 0:36], [(h2T[:, c, :], Wr[:, c, :]) for c in range(8)], [h2Ttag, "Wr"], [prt])
                        lg, lgtag = lg_r.next()
                        rw, rwtag = rw_r.next()
                        A("dve", "tensor_tensor", [prt, "brt"], [lgtag], out=lg[:, 0:36], in0=pr[:, 0:36], in1=brt[:], op=ALU.add)
                        gl = lg[:, 0:4]
                        el = lg[:, 4:36].rearrange("p (g e) -> p g e", g=4)
                        A("dve", "tensor_reduce", [lgtag], [(rwtag, 0)], out=rw[:, 0:1], in_=gl, axis=AX.X, op=ALU.max)
                        A("dve", "tensor_scalar", [(rwtag, 0)], [(rwtag, 1)], out=rw[:, 1:2], in0=rw[:, 0:1], scalar1=-1.0, scalar2=None, op0=ALU.mult)
                        A("dve", "tensor_scalar", [lgtag, (rwtag, 0)], [(rwtag, 8)], out=rw[:, 8:12], in0=gl, scalar1=rw[:, 0:1], scalar2=None, op0=ALU.is_equal)
                        A("act", "activation", [lgtag, (rwtag, 1)], [(rwtag, 2), (rwtag, 44)], out=rw[:, 44:48], in_=gl, func=AF.Exp, bias=rw[:, 1:2], accum_out=rw[:, 2:3])
                        A("dve", "reciprocal", [(rwtag, 2)], [(rwtag, 2)], out=rw[:, 2:3], in_=rw[:, 2:3])
                        l2, l2tag = lg_r.next()
                        A("dve", "tensor_tensor", [lgtag, (rwtag, 8)], [l2tag], out=l2[:, 0:32].rearrange("p (g e) -> p g e", g=4), in0=el,
                          in1=rw[:, 8:12].unsqueeze(2).to_broadcast([128, 4, 8]), op=ALU.mult)
                        A("dve", "tensor_reduce", [l2tag], [(rwtag, 12)], out=rw[:, 12:20], in_=l2[:, 0:32].rearrange("p (g e) -> p e g", g=4), axis=AX.X, op=ALU.add)
                        A("dve", "max", [(rwtag, 12)], [(rwtag, 20)], out=rw[:, 20:28], in_=rw[:, 12:20])
                        A("dve", "tensor_scalar", [(rwtag, 12), (rwtag, 20)], [(rwtag, 28)], out=rw[:, 28:36], in0=rw[:, 12:20], scalar1=rw[:, 20:21], scalar2=None, op0=ALU.is_equal)
                        A("dve", "tensor_scalar", [(rwtag, 12), (rwtag, 20)], [(rwtag, 36)], out=rw[:, 36:44], in0=rw[:, 12:20], scalar1=rw[:, 21:22], scalar2=None, op0=ALU.is_equal)
                        for (Mall, mname, c0) in ((M1all, "M1", 28), (M2all, "M2", 36)):
                            A("dve", "tensor_tensor", [(rwtag, 8), (rwtag, c0)], [(mname, t)], out=Mall[:, t, :].rearrange("p (g e) -> p g e", g=4),
                              in0=rw[:, 8:12].unsqueeze(2).to_broadcast([128, 4, 8]), in1=rw[:, c0:c0 + 8].unsqueeze(1).to_broadcast([128, 4, 8]), op=ALU.mult)
                        A("dve", "tensor_tensor", [(rwtag, 20)], [(rwtag, 3)], out=rw[:, 3:4], in0=rw[:, 21:22], in1=rw[:, 20:21], op=ALU.subtract)
                        A("act", "activation", [(rwtag, 3)], [(rwtag, 4)], out=rw[:, 4:5], in_=rw[:, 3:4], func=AF.Exp)
                        A("dve", "tensor_scalar", [(rwtag, 4)], [(rwtag, 5)], out=rw[:, 5:6], in0=rw[:, 4:5], scalar1=1.0, scalar2=None, op0=ALU.add)
                        A("dve", "reciprocal", [(rwtag, 5)], [(rwtag, 5)], out=rw[:, 5:6], in_=rw[:, 5:6])
                        A("dve", "tensor_tensor", [(rwtag, 5), (rwtag, 2)], [("wts", t)], out=wts[:, t, 0:1], in0=rw[:, 5:6], in1=rw[:, 2:3], op=ALU.mult)
                        A("dve", "tensor_tensor", [("wts", t), (rwtag, 4)], [("wts", t)], out=wts[:, t, 1:2], in0=wts[:, t, 0:1], in1=rw[:, 4:5], op=ALU.mult)
                    S.auto_pipeline(4, o_item)
                S.barrier()
            PS2.close()

            with contextlib.ExitStack() as PB:
                pba = ps_alloc_in(PB)

                def balloc(name, shape, dt):
                    return PB.enter_context(nc.sbuf_tensor("s_r_" + name, shape, dt))
                rank_ps = pba("r_rank", [128, 512], F32)
                tot_ps = pba("r_tot", [128, 32], F32)
                Ub = balloc("U", [128, 128], BF)
                onesb = balloc("ones", [128, 128], BF)
                thr = balloc("thr", [128, 16], F32)
                ltm = balloc("ltm", [128, 32, 32], F32)
                evec = balloc("evec", [128, 32], F32)
                kvec = balloc("kvec", [128, NT], F32)
                pidx = balloc("pidx", [128, 1], F32)
                S.dma("pool", Ub[:], U_d, writes=["U"])
                S.dma("pool", onesb[:], ones_d, writes=["ones"])
                S.dma("sp", thr[:], thr_d.partition_broadcast(128), writes=["thr"])
                S.dma("sp", ltm[:].rearrange("p a b -> p (a b)"), ltm_d.partition_broadcast(128), writes=["ltm"])
                S.dma("sp", evec[:], evec_d.partition_broadcast(128), writes=["evec"])
                S.dma("sp", kvec[:], kvec_d.partition_broadcast(128), writes=["kvec"])
                S.dma("sp", pidx[:], pidx_d, writes=["pidx"])
                Mb = balloc("Mb", [128, 16, 32], BF)
                mtags = [("M1", t) for t in range(16)] + [("M2", t) for t in range(16)]
                A("dve", "tensor_tensor", mtags, ["Mb"], out=Mb[:], in0=M1all[:], in1=M2all[:], op=ALU.add)

                def rank_fn(e):
                    first = True
                    ins = None
                    for t in range(16):
                        lst = [(Ub, t)] + [(onesb, t2) for t2 in range(t)]
                        for i, (l, tt) in enumerate(lst):
                            ins = e.matmul(rank_ps[:, t * 32:(t + 1) * 32], lhsT=l[:], rhs=Mb[:, tt, :], start=first,
                                           stop=(t == 15 and i == len(lst) - 1))
                            first = False
                    return ins
                S.op("pe", rank_fn, ["Mb", "U", "ones"], ["rank_ps"])
                MM(tot_ps[:, :], [(onesb[:], Mb[:, t, :]) for t in range(16)], ["Mb", "ones"], ["tot_ps"])
                n_sb = balloc("n", [128, 32], F32)
                A("dve", "tensor_copy", ["tot_ps"], ["n"], out=n_sb[:], in_=tot_ps[:, :])
                cmp = balloc("cmp", [128, 32, 16], F32)
                A("dve", "tensor_tensor", ["n", "thr"], ["cmp"], out=cmp[:], in0=n_sb[:].unsqueeze(2).to_broadcast([128, 32, 16]),
                  in1=thr[:].unsqueeze(1).to_broadcast([128, 32, 16]), op=ALU.is_gt)
                ntl = balloc("ntl", [128, 32], F32)
                A("dve", "tensor_reduce", ["cmp"], ["ntl"], out=ntl[:], in_=cmp[:], axis=AX.X, op=ALU.add)
                tmp = balloc("tmp", [128, 32, 32], F32)
                A("dve", "tensor_tensor", ["ntl", "ltm"], ["tmp"], out=tmp[:], in0=ltm[:], in1=ntl[:].unsqueeze(1).to_broadcast([128, 32, 32]), op=ALU.mult)
                tst = balloc("tst", [128, 32], F32)
                A("dve", "tensor_reduce", ["tmp"], ["tst"], out=tst[:], in_=tmp[:], axis=AX.X, op=ALU.add)
                tend = balloc("tend", [128, 32], F32)
                A("dve", "tensor_tensor", ["tst", "ntl"], ["tend"], out=tend[:], in0=tst[:], in1=ntl[:], op=ALU.add)
                sbase = balloc("sbase", [128, 32], F32)
                A("dve", "tensor_scalar", ["tst"], ["sbase"], out=sbase[:], in0=tst[:], scalar1=256.0, scalar2=None, op0=ALU.mult)
                slotm = balloc("slotm", [128, 16, 32], F32)
                A("dve", "tensor_tensor", ["rank_ps", "sbase"], ["slotm"], out=slotm[:], in0=rank_ps[:, :].rearrange("p (t e) -> p t e", t=16),
                  in1=sbase[:].unsqueeze(1).to_broadcast([128, 16, 32]), op=ALU.add)
                prod = balloc("prod", [128, 16, 32], F32)
                slotf = balloc("slotf", [128, 2, 16], F32)
                for j, (Mall, mname) in enumerate(((M1all, "M1"), (M2all, "M2"))):
                    A("dve", "tensor_tensor", ["slotm"] + [(mname, t) for t in range(16)], ["prod"], out=prod[:], in0=slotm[:], in1=Mall[:], op=ALU.mult)
                    A("dve", "tensor_reduce", ["prod"], [("slotf", j)], out=slotf[:, j, :], in_=prod[:], axis=AX.X, op=ALU.add)
                A("dve", "tensor_copy", [("slotf", 0), ("slotf", 1)], ["slot_i"], out=slot_i[:], in_=slotf[:].rearrange("p j t -> p (j t)"))
                ind = balloc("ind", [128, NT, 32], F32)
                ind2 = balloc("ind2", [128, NT, 32], F32)
                A("dve", "tensor_tensor", ["tst", "kvec"], ["ind"], out=ind[:], in0=tst[:].unsqueeze(1).to_broadcast([128, NT, 32]),
                  in1=kvec[:].unsqueeze(2).to_broadcast([128, NT, 32]), op=ALU.is_le)
                A("dve", "tensor_tensor", ["tend", "kvec"], ["ind2"], out=ind2[:], in0=tend[:].unsqueeze(1).to_broadcast([128, NT, 32]),
                  in1=kvec[:].unsqueeze(2).to_broadcast([128, NT, 32]), op=ALU.is_gt)
                A("dve", "tensor_tensor", ["ind", "ind2"], ["ind"], out=ind[:], in0=ind[:], in1=ind2[:], op=ALU.mult)
                used = balloc("used", [128, NT], F32)
                A("dve", "tensor_reduce", ["ind"], ["used"], out=used[:], in_=ind[:], axis=AX.X, op=ALU.add)
                A("dve", "tensor_tensor", ["ind", "evec"], ["ind2"], out=ind2[:], in0=ind[:], in1=evec[:].unsqueeze(1).to_broadcast([128, NT, 32]), op=ALU.mult)
                ek = balloc("ek", [128, NT], F32)
                A("dve", "tensor_reduce", ["ind2"], ["ek"], out=ek[:], in_=ind2[:], axis=AX.X, op=ALU.add)
                A("dve", "tensor_scalar", ["ek", "pidx"], ["ek"], out=ek[:], in0=ek[:], scalar1=128.0, scalar2=pidx[:, 0:1], op0=ALU.mult, op1=ALU.add)
                A("dve", "tensor_scalar", ["ek"], ["ek"], out=ek[:], in0=ek[:], scalar1=-BIG, scalar2=None, op0=ALU.add)
                A("dve", "tensor_tensor", ["ek", "used"], ["ek"], out=ek[:], in0=ek[:], in1=used[:], op=ALU.mult)
                A("dve", "tensor_scalar", ["ek"], ["ek"], out=ek[:], in0=ek[:], scalar1=BIG, scalar2=None, op0=ALU.add)
                A("dve", "tensor_copy", ["ek"], ["widx_i"], out=widx_i[:], in_=ek[:])
                if debug:
                    finals.append(S.dma("sp", dbg["slot"], slot_i[:], reads=["slot_i"]))
                    finals.append(S.dma("sp", dbg["widx"], widx_i[:], reads=["widx_i"]))
                S.barrier()

            with contextlib.ExitStack() as P:
                def palloc(name, shape, dt):
                    return P.enter_context(nc.sbuf_tensor("s_d_" + name, shape, dt))
                psa = ps_alloc_in(P)
                Wt_r = Ring(palloc, "Wt", 6, [128, 6144], BF)
                xtok_r = Ring(palloc, "xtok", 2, [128, 2, 1024], BF)
                xT_r = Ring(palloc, "xT", 2, [128, 8, 256], BF)
                sl_r = Ring(palloc, "sl", 4, [128, 2, 256], F32)
                hid_r = Ring(palloc, "hid", 2, [128, 2, 256], BF)
                ysb_r = Ring(palloc, "ysb", 2, [128, 2, 1024], F32)
                pT3 = Ring(psa, "d_pT", 2, [128, 1024], BF)
                pgu_r = Ring(psa, "d_gu", 2, [128, 512], F32)
                psy_r = Ring(psa, "d_y", 4, [128, 512], F32)
                sc_tags = []
                bc_reg = {}

                def issue_wgather(k):
                    Wt, Wtag = Wt_r.next()
                    for q in range(WSPLIT):
                        w0, w1 = q * (6144 // WSPLIT), (q + 1) * (6144 // WSPLIT)

                        def wg(e, w0=w0, w1=w1, q=q):
                            if "r" not in bc_reg:
                                bc_reg["r"] = e.to_reg(4095)
                            return e.indirect_dma_start(out=Wt[:, w0:w1], out_offset=None, in_=W_all[q],
                                                        in_offset=bass.IndirectOffsetOnAxis(ap=widx_i[:, k:k + 1], axis=0),
                                                        bounds_check=bc_reg["r"], oob_is_err=False)
                        S.op("pool", wg, reads=["widx_i"], writes=[(Wtag, q)], dma=True)
                    return Wt, Wtag
                NPRE = 6
                pre_w = [issue_wgather(k) for k in range(NPRE)]
                for t in range(16):
                    for j in range(2):
                        col = j * 16 + t
                        tg = ("xs_scr", col)
                        sc_tags.append(tg)
                        S.op("pool", lambda e, t=t, col=col: e.indirect_dma_start(
                            out=xs_scr, out_offset=bass.IndirectOffsetOnAxis(ap=slot_i[:, col:col + 1], axis=0),
                            in_=h2all[:, t, :], in_offset=None), reads=["slot_i", ("h2", t)], writes=[tg], dma=True)

                def tile_item(k):
                    Wt, Wtag = pre_w[k] if k < NPRE else issue_wgather(k)
                    Wtags = [(Wtag, q) for q in range(WSPLIT)]
                    xtok, xtag = xtok_r.next()
                    S.dma("sp", xtok[:], xs_scr[256 * k:256 * (k + 1), :].rearrange("(s p) d -> p s d", p=128), reads=sc_tags, writes=[xtag])
                    xT, xTtag = xT_r.next()
                    for s_ in range(2):
                        pT, pTtag = pT3.next()
                        TR([(pT[:, c * 128:(c + 1) * 128], xtok[:, s_, c * 128:(c + 1) * 128]) for c in range(8)], [xtag], [pTtag])
                        evac("act" if s_ == 0 else "dve", [pTtag], [(xTtag, s_)], xT[:, :, s_ * 128:(s_ + 1) * 128],
                             pT[:, :].rearrange("p (c t) -> p c t", c=8))
                    xTt = [(xTtag, 0), (xTtag, 1)]
                    hid, hidtag = hid_r.next()
                    for f in range(2):
                        pgu, pgutag = pgu_r.next()
                        MM(pgu[:, 0:256], [(Wt[:, c * 256 + f * 128:c * 256 + (f + 1) * 128], xT[:, c, :]) for c in range(8)], Wtags + xTt, [pgutag])
                        MM(pgu[:, 256:512], [(Wt[:, 2048 + c * 256 + f * 128:2048 + c * 256 + (f + 1) * 128], xT[:, c, :]) for c in range(8)], Wtags + xTt, [pgutag])
                        sl, sltag = sl_r.next()
                        A("act", "activation", [pgutag], [sltag], out=sl[:, 0, :], in_=pgu[:, 0:256], func=AF.Silu)
                        A("act", "copy", [pgutag], [sltag], out=sl[:, 1, :], in_=pgu[:, 256:512])
                        A("dve", "tensor_tensor", [sltag], [(hidtag, f)], out=hid[:, f, :], in0=sl[:, 0, :], in1=sl[:, 1, :], op=ALU.mult)
                    ysb, ytag = ysb_r.next()
                    for sh in range(2):
                        for u in range(2):
                            py, pytag = psy_r.next()
                            MM(py[:, :], [(hid[:, f, sh * 128:(sh + 1) * 128], Wt[:, 4096 + f * 1024 + u * 512:4096 + f * 1024 + (u + 1) * 512]) for f in range(2)],
                               [(hidtag, 0), (hidtag, 1)] + Wtags, [pytag])
                            evac("act" if u == 0 else "dve", [pytag], [(ytag, sh, u)], ysb[:, sh, u * 512:(u + 1) * 512], py[:, :])
                    S.dma("sp", y_scr[256 * k:256 * (k + 1), :].rearrange("(s p) d -> p s d", p=128), ysb[:],
                          reads=[(ytag, sh, u) for sh in range(2) for u in range(2)], writes=[("y_scr", k)])
                nlv = S.auto_pipeline(NT, tile_item)
                if VERBOSE:
                    print("moe tile levels", nlv)
                S.barrier()
            with contextlib.ExitStack() as P:
                def palloc(name, shape, dt):
                    return P.enter_context(nc.sbuf_tensor("s_f_" + name, shape, dt))
                y1_r = Ring(palloc, "y1", 3, [128, 1024], F32)
                y2_r = Ring(palloc, "y2", 3, [128, 1024], F32)
                xf_r = Ring(palloc, "xf", 3, [128, 1024], F32)

                def fin_item(t):
                    y1, y1tag = y1_r.next()
                    y2, y2tag = y2_r.next()
                    xf, xftag = xf_r.next()
                    for (yy, yytag, col) in ((y1, y1tag, t), (y2, y2tag, 16 + t)):
                        S.op("pool", lambda e, yy=yy, col=col: e.indirect_dma_start(
                            out=yy[:], out_offset=None, in_=y_scr, in_offset=bass.IndirectOffsetOnAxis(ap=slot_i[:, col:col + 1], axis=0)),
                            reads=["slot_i"], writes=[yytag], dma=True)
                    S.dma("sp", xf[:], xmid_d[t * 128:(t + 1) * 128, :], writes=[xftag])
                    A("dve", "scalar_tensor_tensor", [y1tag, ("wts", t), xftag], [xftag], out=xf[:], in0=y1[:], scalar=wts[:, t, 0:1], in1=xf[:], op0=ALU.mult, op1=ALU.add)
                    A("dve", "scalar_tensor_tensor", [y2tag, ("wts", t), xftag], [xftag], out=xf[:], in0=y2[:], scalar=wts[:, t, 1:2], in1=xf[:], op0=ALU.mult, op1=ALU.add)
                    finals.append(S.dma("sp", out_d[t * 128:(t + 1) * 128, :], xf[:], reads=[xftag]))
                S.auto_pipeline(16, fin_item)
                S.barrier()
        S.emit(final_waits=finals)
    return nc


def _rope_cs(pos, rot):
    inv = (1.0 / (np.float32(500000.0) ** (np.arange(0, rot, 2, dtype=np.float32) / np.float32(rot)))).astype(np.float32)
    ang = pos.astype(np.float32)[:, None] * inv[None, :]
    return np.concatenate([np.cos(ang), np.sin(ang)], axis=1).astype(np.float32)


def _core_layout(h):
    own = np.concatenate([np.arange((2 * j + h) * 512, (2 * j + h) * 512 + 512) for j in range(4)])
    m = np.zeros((8, 128, 512), np.float32)
    for mi in range(8):
        kpos = mi * 128 + np.arange(128)
        qpos = h * 512 + np.arange(512)
        m[mi] = ((kpos[:, None] // 64) <= (qpos[None, :] // 64)).astype(np.float32)
    return own, m


def _moe_weight_rows(wg, wu, wd):
    g = wg.reshape(32, 8, 128, 256).transpose(0, 2, 1, 3).reshape(32, 128, 2048)
    u = wu.reshape(32, 8, 128, 256).transpose(0, 2, 1, 3).reshape(32, 128, 2048)
    d = wd.reshape(32, 2, 128, 1024).transpose(0, 2, 1, 3).reshape(32, 128, 2048)
    full = np.concatenate([g, u, d], axis=2).reshape(4096, 6144)
    wq = 6144 // WSPLIT
    return {f"W_all{q}": np.ascontiguousarray(full[:, q * wq:(q + 1) * wq]) for q in range(WSPLIT)}


_NC_CACHE = {}


def kernel(**inputs):
    f = lambda k: np.ascontiguousarray(np.asarray(inputs[k], dtype=np.float32))
    x = f("x")
    if "nc" not in _NC_CACHE:
        _NC_CACHE["nc"] = build(DEBUG)
    nc = _NC_CACHE["nc"]
    pos = np.arange(4096)
    shared = dict(
        ident=np.eye(128, dtype=np.float32),
        w_in=f("w_in")[0], w_uq=f("w_mla_uq")[0], w_ukv=f("w_mla_ukv")[0], w_mup=f("w_mla_up")[0], w_dup=f("w_diff_up")[0],
        w_out=f("w_out")[0],
        w_rt=np.ascontiguousarray(np.concatenate([f("w_router_group")[0], f("w_router_expert")[0]], axis=1)),
        b_rt=np.ascontiguousarray(np.concatenate([f("b_router_group")[0], f("b_router_expert")[0]])[None, :]),
        **_moe_weight_rows(f("w_expert_gate")[0], f("w_expert_up")[0], f("w_expert_down")[0]),
        U_tri=np.triu(np.ones((128, 128), np.float32), 1), ones_m=np.ones((128, 128), np.float32),
        thr=(256.0 * np.arange(16, dtype=np.float32))[None, :],
        ltm=np.tril(np.ones((32, 32), np.float32), -1).reshape(1, 1024),
        evec=np.arange(32, dtype=np.float32)[None, :], kvec=np.arange(NT, dtype=np.float32)[None, :],
        pidx=np.arange(128, dtype=np.float32)[:, None],
        norm_mix=f("norm_mix"), qlat_norm=f("mla_q_latent_norm"), kvlat_norm=f("mla_kv_latent_norm"), mq_gain=f("mla_q_gain"),
        mk_gain=f("mla_k_gain"), dq_gain=f("diff_q_gain"), dk_gain=f("diff_k_gain"), lq1=f("lambda_q1"), lk1=f("lambda_k1"),
        lq2=f("lambda_q2"), lk2=f("lambda_k2"), subln=f("diff_subln"), norm_ffn=f("norm_ffn"),
        csk_m=_rope_cs(pos, 32), csk_d=_rope_cs(pos, 16),
    )
    in_maps = []
    owns = []
    for c in range(8):
        b, h = c // 2, c % 2
        own, m = _core_layout(h)
        owns.append(own)
        d = dict(shared)
        d["xs"] = x[b]
        d["xo"] = np.ascontiguousarray(x[b][own])
        d["csq_m"] = _rope_cs(own, 32)
        d["csq_d"] = _rope_cs(own, 16)
        d["masks"] = m
        in_maps.append(d)
    if _NC_CACHE.get("prepare_only"):
        return nc, in_maps, owns
    res = run_bass_kernel_spmd(nc, in_maps, core_ids=list(range(8)))
    out = np.empty((4, 4096, 1024), np.float32)
    for c in range(8):
        out[c // 2][owns[c]] = res.results[c]["out"]
    kernel.last_results = res.results
    return out
```
